# Optimizing a Trainium2 kernel written in Bass

```python
import math
import jax, jax.numpy as jnp
from jax import lax
import numpy as np

D_MODEL = 2048
BATCH = 4
SEQ = 4096
DEPTH = 1

CHUNK = 64
Q_BLOCK = 128
N_HEADS = 8
HEAD_DIM = 64
ATT_WIDTH = N_HEADS * 2 * HEAD_DIM
CONV_CHANNELS = 1024
CONV_TAPS = 31
N_BUCKETS = 32
MAX_DISTANCE = 128
PEER_HEADS = 8
PEER_N_KEYS = 128
PEER_N_EXPERTS = PEER_N_KEYS * PEER_N_KEYS
PEER_HALF_DIM = 128
PEER_QUERY_DIM = 2 * PEER_HALF_DIM
PEER_TOPK = 16
PEER_TOKEN_BLOCK = 128
IN_COLS = 3 * ATT_WIDTH + 2 * CONV_CHANNELS + 2 * D_MODEL
EPS = 1e-6
NEG = -1e30

kernel_name = "hybrid_diffattn_conformer_peer_block"


def rms_norm(x, g):
    xf = x.astype(jnp.float32)
    y = xf * lax.rsqrt(jnp.mean(xf * xf, axis=-1, keepdims=True) + EPS)
    return (y * g.astype(jnp.float32)).astype(x.dtype)


def layer_norm(x, g, b):
    xf = x.astype(jnp.float32)
    mu = jnp.mean(xf, axis=-1, keepdims=True)
    xc = xf - mu
    var = jnp.mean(xc * xc, axis=-1, keepdims=True)
    y = xc * lax.rsqrt(var + EPS) * g.astype(jnp.float32) + b.astype(jnp.float32)
    return y.astype(x.dtype)


def rel_bucket(rel):
    nb = N_BUCKETS // 2
    max_exact = nb // 2
    ret = (rel > 0).astype(jnp.int32) * nb
    n = jnp.abs(rel)
    nf = jnp.maximum(n, 1).astype(jnp.float32)
    large = max_exact + (jnp.log(nf / max_exact) / math.log(MAX_DISTANCE / max_exact)
                         * (nb - max_exact)).astype(jnp.int32)
    large = jnp.minimum(large, nb - 1)
    return ret + jnp.where(n < max_exact, n, large)


def diff_attention(q, k, v, lam, rel_bias):
    S = q.shape[1]
    q = q * (HEAD_DIM ** -0.5)
    outs = []
    for i in range(S // Q_BLOCK):
        q0 = i * Q_BLOCK
        end = q0 + Q_BLOCK
        qb = q[:, q0:end]
        kb = k[:, :end]
        vb = v[:, :end]
        logits = jnp.einsum('bqhmd,bkhmd->bmhqk', qb, kb).astype(jnp.float32)
        q_pos = q0 + jnp.arange(Q_BLOCK, dtype=jnp.int32)
        k_pos = jnp.arange(end, dtype=jnp.int32)
        bias = rel_bias[rel_bucket(k_pos[None, :] - q_pos[:, None])]
        bias = jnp.transpose(bias, (2, 0, 1)).astype(jnp.float32)
        mask = (k_pos[None, :] // CHUNK) <= (q_pos[:, None] // CHUNK)
        logits = jnp.where(mask, logits + bias, NEG)
        p = jax.nn.softmax(logits, axis=-1)
        w = p[:, 0] - lam * p[:, 1]
        outs.append(jnp.einsum('bhqk,bkhe->bqhe', w.astype(v.dtype), vb))
    return jnp.concatenate(outs, axis=1)


def conformer_conv(glu_in, conv_w, conv_b, ln_g, ln_b, w_co):
    a, b = jnp.split(glu_in, 2, axis=-1)
    h = a * jax.nn.sigmoid(b)
    h = lax.conv_general_dilated(
        h, conv_w[:, None, :], window_strides=(1,), padding=[(CONV_TAPS - 1, 0)],
        dimension_numbers=('NWC', 'WIO', 'NWC'), feature_group_count=CONV_CHANNELS) + conv_b
    h = layer_norm(h, ln_g, ln_b)
    h = h * jax.nn.sigmoid(h)
    return h @ w_co


def peer(h, w_q, sub_keys, u_tab, v_tab):
    B, S, D = h.shape
    T = B * S
    ht = h.reshape(T, D)
    q = (ht @ w_q).reshape(T, PEER_HEADS, 2, PEER_HALF_DIM)
    scores = jnp.einsum('thpc,hpnc->thpn', q, sub_keys).astype(jnp.float32)
    s_half, i_half = lax.top_k(scores, PEER_TOPK)
    cand_s = s_half[:, :, 0, :, None] + s_half[:, :, 1, None, :]
    cand_i = i_half[:, :, 0, :, None] * PEER_N_KEYS + i_half[:, :, 1, None, :]
    n_cand = PEER_TOPK * PEER_TOPK
    top_s, pos = lax.top_k(cand_s.reshape(T, PEER_HEADS, n_cand), PEER_TOPK)
    idx = jnp.take_along_axis(cand_i.reshape(T, PEER_HEADS, n_cand), pos, axis=-1)
    g = jax.nn.softmax(top_s, axis=-1).astype(h.dtype)
    nb = T // PEER_TOKEN_BLOCK
    hk = PEER_HEADS * PEER_TOPK
    xs = (ht.reshape(nb, PEER_TOKEN_BLOCK, D),
          idx.reshape(nb, PEER_TOKEN_BLOCK, hk),
          g.reshape(nb, PEER_TOKEN_BLOCK, hk))

    def apply_block(args):
        xb, ib, gb = args
        a = jnp.einsum('tnd,td->tn', u_tab[ib], xb)
        wts = gb * jax.nn.gelu(a, approximate=False)
        return jnp.einsum('tn,tnd->td', wts, v_tab[ib])

    y = lax.map(apply_block, xs)
    return y.reshape(B, S, D)


def setup_inputs(seed: int = 0) -> dict:
    key = jax.random.key(seed)
    ks = jax.random.split(key, 24)

    def nrm(k, shape, scale):
        return jax.random.normal(k, shape, jnp.float32) * scale

    return {
        "x": nrm(ks[0], (BATCH, SEQ, D_MODEL), 1.0),
        "norm_mix_g": 1.0 + nrm(ks[1], (DEPTH, D_MODEL), 0.02),
        "w_in": nrm(ks[2], (DEPTH, D_MODEL, IN_COLS), D_MODEL ** -0.5),
        "b_gate": nrm(ks[3], (DEPTH, 2 * D_MODEL), 0.01),
        "lam_q1": nrm(ks[4], (DEPTH, HEAD_DIM), 0.1),
        "lam_k1": nrm(ks[5], (DEPTH, HEAD_DIM), 0.1),
        "lam_q2": nrm(ks[6], (DEPTH, HEAD_DIM), 0.1),
        "lam_k2": nrm(ks[7], (DEPTH, HEAD_DIM), 0.1),
        "subln_g": 1.0 + nrm(ks[8], (DEPTH, 2 * HEAD_DIM), 0.02),
        "w_att_out": nrm(ks[9], (DEPTH, ATT_WIDTH, D_MODEL), ATT_WIDTH ** -0.5),
        "conv_w": nrm(ks[10], (DEPTH, CONV_TAPS, CONV_CHANNELS), CONV_TAPS ** -0.5),
        "conv_b": nrm(ks[11], (DEPTH, CONV_CHANNELS), 0.01),
        "conv_ln_g": 1.0 + nrm(ks[12], (DEPTH, CONV_CHANNELS), 0.02),
        "conv_ln_b": nrm(ks[13], (DEPTH, CONV_CHANNELS), 0.01),
        "w_conv_out": nrm(ks[14], (DEPTH, CONV_CHANNELS, D_MODEL), CONV_CHANNELS ** -0.5),
        "w_out": nrm(ks[15], (DEPTH, D_MODEL, D_MODEL), D_MODEL ** -0.5),
        "rel_bias": nrm(ks[16], (N_BUCKETS, N_HEADS), 0.5),
        "norm_ffn_g": 1.0 + nrm(ks[17], (DEPTH, D_MODEL), 0.02),
        "peer_w_q": nrm(ks[18], (DEPTH, D_MODEL, PEER_HEADS * PEER_QUERY_DIM), D_MODEL ** -0.5),
        "peer_sub_keys": nrm(ks[19], (DEPTH, PEER_HEADS, 2, PEER_N_KEYS, PEER_HALF_DIM), PEER_HALF_DIM ** -0.5),
        "peer_u": nrm(ks[20], (DEPTH, PEER_N_EXPERTS, D_MODEL), D_MODEL ** -0.5),
        "peer_v": nrm(ks[21], (DEPTH, PEER_N_EXPERTS, D_MODEL), PEER_HEADS ** -0.5),
        "final_norm_g": 1.0 + nrm(ks[22], (D_MODEL,), 0.02),
    }


def reference(x, norm_mix_g, w_in, b_gate, lam_q1, lam_k1, lam_q2, lam_k2, subln_g,
              w_att_out, conv_w, conv_b, conv_ln_g, conv_ln_b, w_conv_out, w_out,
              rel_bias, norm_ffn_g, peer_w_q, peer_sub_keys, peer_u, peer_v, final_norm_g):
    B, S, _ = x.shape
    h = x
    cuts = [ATT_WIDTH, 2 * ATT_WIDTH, 3 * ATT_WIDTH, 3 * ATT_WIDTH + 2 * CONV_CHANNELS]
    for l in range(DEPTH):
        lam_init = 0.8 - 0.6 * math.exp(-0.3 * l)
        n = rms_norm(h, norm_mix_g[l])
        proj = n @ w_in[l]
        q, k, v, glu_in, gate_logits = jnp.split(proj, cuts, axis=-1)
        q = q.reshape(B, S, N_HEADS, 2, HEAD_DIM)
        k = k.reshape(B, S, N_HEADS, 2, HEAD_DIM)
        v = v.reshape(B, S, N_HEADS, 2 * HEAD_DIM)
        lam = (jnp.exp(jnp.sum(lam_q1[l].astype(jnp.float32) * lam_k1[l].astype(jnp.float32)))
               - jnp.exp(jnp.sum(lam_q2[l].astype(jnp.float32) * lam_k2[l].astype(jnp.float32)))
               + lam_init)
        o = diff_attention(q, k, v, lam, rel_bias)
        o = rms_norm(o, subln_g[l]) * (1.0 - lam_init)
        y_att = o.reshape(B, S, ATT_WIDTH) @ w_att_out[l]
        y_conv = conformer_conv(glu_in, conv_w[l], conv_b[l], conv_ln_g[l],
                                conv_ln_b[l], w_conv_out[l])
        g_att, g_conv = jnp.split(jax.nn.sigmoid(gate_logits + b_gate[l]), 2, axis=-1)
        h = h + (g_att * y_att + g_conv * y_conv) @ w_out[l]
        h = h + peer(rms_norm(h, norm_ffn_g[l]), peer_w_q[l], peer_sub_keys[l],
                     peer_u[l], peer_v[l])
    return rms_norm(h, final_norm_g)
```

```python
import math
import numpy as np
import ml_dtypes
from contextlib import ExitStack
import concourse.bass as bass
import concourse.mybir as mybir
from concourse.bass_utils import run_bass_kernel_spmd

F32 = mybir.dt.float32
BF16 = mybir.dt.bfloat16
U32 = mybir.dt.uint32
AF = mybir.ActivationFunctionType
ALU = mybir.AluOpType
AX = mybir.AxisListType

D = 2048
T = 2048
NEG = -1e30
EPS = 1e-6
LAM_INIT = 0.8 - 0.6 * math.exp(0.0)


class Res:
    def __init__(self, sem, step):
        self.sem = sem
        self.step = step
        self.count = 0


class Eng:
    def __init__(self, name, h, res):
        self.name = name
        self.h = h
        self.res = res
        self.seen = {}


class Tracker:
    def __init__(self, nc, es):
        self.nc = nc
        self.es = es
        self.last_write = {}
        self.readers = {}
        self.engs = {}
        for name, h in (("pe", nc.tensor), ("act", nc.scalar), ("dve", nc.vector),
                        ("pool", nc.gpsimd), ("sp", nc.sync)):
            sem = es.enter_context(nc.semaphore("sem_" + name))
            self.engs[name] = Eng(name, h, Res(sem, 1))
        self.all_res = [e.res for e in self.engs.values()]
        self.nslots = 0
        self.bg = set()

    def slot(self, name):
        sem = self.es.enter_context(self.nc.semaphore("dq_" + name))
        r = Res(sem, 16)
        self.all_res.append(r)
        self.nslots += 1
        return r

    def _need(self, eng, stamp):
        res, val = stamp
        if eng.seen.get(res, 0) >= val:
            return
        eng.h.wait_ge(res.sem, val)
        eng.seen[res] = val

    def _deps(self, eng, own, reads, writes):
        for k in reads:
            st = self.last_write.get(k)
            if st is not None:
                self._need(eng, st)
        for k in writes:
            st = self.last_write.get(k)
            if st is not None and st[0] is not own:
                self._need(eng, st)
            for r, v in self.readers.get(k, {}).items():
                if r is not own:
                    self._need(eng, (r, v))

    def _commit(self, stamp, reads, writes):
        for k in reads:
            d = self.readers.setdefault(k, {})
            d[stamp[0]] = max(d.get(stamp[0], 0), stamp[1])
        for k in writes:
            self.last_write[k] = stamp
            self.readers[k] = {}

    def op(self, ename, fn, reads=(), writes=()):
        eng = self.engs[ename]
        self._deps(eng, eng.res, reads, writes)
        ins = fn(eng.h)
        eng.res.count += 1
        ins.then_inc(eng.res.sem, 1)
        self._commit((eng.res, eng.res.count), reads, writes)
        return ins

    def dma(self, qname, slot, out, in_, reads=(), writes=(), **kw):
        eng = self.engs[qname]
        self._deps(eng, None, reads, writes)
        ins = eng.h.dma_start(out=out, in_=in_, **kw)
        slot.count += 16
        ins.then_inc(slot.sem, 16)
        self._commit((slot, slot.count), reads, writes)
        return ins

    def barrier(self, join_bg=False):
        if join_bg:
            self.bg = set()
        for eng in self.engs.values():
            for r in self.all_res:
                if r.count > 0 and r not in self.bg:
                    self._need(eng, (r, r.count))
        keep_w = {k: v for k, v in self.last_write.items() if v[0] in self.bg}
        self.last_write = keep_w
        self.readers = {}

    def finish(self, ename="sp"):
        eng = self.engs[ename]
        for r in self.all_res:
            if r.count > 0:
                self._need(eng, (r, r.count))


class Ring:
    def __init__(self, tr, alloc, name, n, shape, dt, slots=True):
        self.bufs = [alloc(f"{name}{i}", shape, dt) for i in range(n)]
        self.keys = [f"{name}{i}" for i in range(n)]
        self.slots = [tr.slot(f"{name}{i}") for i in range(n)] if slots else None
        self.i = -1
        self.n = n

    def next(self):
        self.i = (self.i + 1) % self.n
        return self.cur()

    def cur(self):
        return self.bufs[self.i], self.keys[self.i], (self.slots[self.i] if self.slots else None)


def bcast_rows(ap1d, n):
    return bass.AP(ap1d.tensor, ap1d.offset, [[0, 128], [1, n]])


def build(upto=99, debug=()):
    nc = bass.Bass("TRN2", target_bir_lowering=False)

    def din(name, shape, dt=F32):
        return nc.dram_tensor(name, list(shape), dt, kind="ExternalInput").ap()

    def scratch(name, shape, dt):
        kind = "ExternalOutput" if name in debug else "Internal"
        return nc.dram_tensor(name, list(shape), dt, kind=kind).ap()

    SHAPES = {
        "x_own": [T, D], "x_pre": [T, D], "w_in": [36, 128, 16, 256], "norm_mix_g": [D], "b_gate": [128, 32],
        "lam4": [4, 64], "subln_g": [128], "w_att_out": [8, 128, 8, 256], "conv_w": [128, 8, 31], "cvec_in": [128, 3, 8],
        "w_conv_out": [8, 128, 8, 256], "w_out": [D, D],
        "rel_bias": [32, 8], "norm_ffn_g": [D], "peer_w_q": [D, D], "peer_sub_keys": [16, 128, 128],
        "peer_u": [16384, D], "peer_v": [16384, D], "final_norm_g": [D], "oh_r": [32, 512],
        "iota128": [128, 128], "flags": [128, 2],
    }
    _decl = {}

    def I(name):
        if name not in _decl:
            _decl[name] = din(name, SHAPES[name])
        return _decl[name]
    nc._used_inputs = _decl
    out = nc.dram_tensor("out", [T, D], F32, kind="ExternalOutput").ap()

    qT_d = scratch("qT_d", [1024, T], BF16)
    kT_d = scratch("kT_d", [1024, 2 * T], BF16)
    v_d = scratch("v_d", [2 * T, 1024], BF16)
    hT_d = scratch("hT_d", [1024, 32 + T], F32)
    gates_d = scratch("gates_d", [4096, T], BF16)
    gr_d = scratch("gr_d", [8, 512], F32)
    oT_d = scratch("oT_d", [1024, T], BF16)
    mixT_d = scratch("mixT_d", [D, T], BF16)
    h1_d = scratch("h1_d", [T, D], F32)
    xn2T_d = scratch("xn2T_d", [16, 128, T], BF16)
    uT_d = scratch("uT_d", [64, 128, 16, 256], BF16)
    vb_d = scratch("vb_d", [16384, D], BF16)

    dbgP = scratch("dbgP", [128, 2, 512], BF16)
    dbgO = scratch("dbgO", [128, 3, 512], F32)
    dbgE = scratch("dbgE", [128, 260], F32)
    with ExitStack() as es:
        es.enter_context(nc.Block())
        tr = Tracker(nc, es)

        def mk_alloc(stack):
            def sb(name, shape, dt):
                return stack.enter_context(nc.sbuf_tensor(name, list(shape), dt))

            def ps(name, shape, dt):
                return stack.enter_context(nc.psum_tensor(name, list(shape), dt))
            return sb, ps

        gsb, gps = mk_alloc(es)
        s_c = tr.slot("const")
        ident_f = gsb("ident_f", [128, 128], F32)
        ident_b = gsb("ident_b", [128, 128], BF16)
        anti_b = gsb("anti_b", [128, 128], BF16)
        iota_f = gsb("iota_f", [128, 128], F32)
        flags = gsb("flags_s", [128, 2], F32)
        ones_b = gsb("ones_b", [128, 128], BF16)
        ones_f = gsb("ones_f", [128, 128], F32)
        tr.op("pool", lambda e: e.memset(ident_f[:], 0.0), writes=["ident_f"])
        tr.op("pool", lambda e: e.affine_select(out=ident_f[:], in_=ident_f[:], pattern=[[-1, 128]],
                                               compare_op=ALU.not_equal, fill=1.0, base=0, channel_multiplier=1),
              reads=["ident_f"], writes=["ident_f"])
        tr.op("dve", lambda e: e.tensor_copy(out=ident_b[:], in_=ident_f[:]), reads=["ident_f"], writes=["ident_b"])
        tr.op("pool", lambda e: e.memset(anti_b[:], 0.0), writes=["anti_b"])
        tr.op("pool", lambda e: e.affine_select(out=anti_b[:], in_=anti_b[:], pattern=[[1, 128]],
                                               compare_op=ALU.not_equal, fill=1.0, base=-127, channel_multiplier=1),
              reads=["anti_b"], writes=["anti_b"])
        tr.op("pool", lambda e: e.memset(ones_b[:], 1.0), writes=["ones_b"])
        tr.op("pool", lambda e: e.memset(ones_f[:], 1.0), writes=["ones_f"])
        tr.dma("sp", s_c, iota_f[:], I("iota128"), writes=["iota_f"])
        tr.dma("sp", s_c, flags[:], I("flags"), writes=["flags"])
        CONST = ["ident_f", "ident_b", "anti_b", "iota_f", "flags", "ones_b", "ones_f"]

        def const_barrier():
            tr.barrier()

        vcast_state = {}

        def vcast_piece(i):
            if "slot" not in vcast_state:
                vcast_state["slot"] = tr.slot("vcast")
                tr.bg.add(vcast_state["slot"])
            for q4 in range(4):
                r0 = i * 1024 + q4 * 256
                tr.dma("pool", vcast_state["slot"], vb_d[r0:r0 + 256, :], I("peer_v")[r0:r0 + 256, :], writes=["vb_d"])

        def make_table_emitter(sb, ps):
            uring = Ring(tr, sb, "u_ld", 2, [128, D], BF16, slots=False)
            ufr = Ring(tr, sb, "u_f32", 2, [128, D], F32)
            stg = Ring(tr, sb, "u_st", 2, [128, 16, 512], BF16)
            ptr = Ring(tr, ps, "u_pt", 2, [128, 8, 128], BF16, slots=False)
            st = {"n": 0, "sg": None}

            def block(eb):
                uf, ufk, ufs = ufr.next()
                tr.dma("sp", ufs, uf[:], I("peer_u")[eb * 128:(eb + 1) * 128, :], writes=[ufk])
                ub, uk, _ = uring.next()
                tr.op("act", lambda e, ub=ub, uf=uf: e.copy(out=ub[:], in_=uf[:]), reads=[ufk], writes=[uk])
                if eb % 4 == 0:
                    st["sg"] = stg.next()
                sg, sk, ss = st["sg"]
                for g in range(4):
                    pt, pk, _ = ptr.next()

                    def tp(e, g=g, ub=ub, pt=pt):
                        for j in range(4):
                            c = g * 4 + j
                            ins = e.transpose(out=pt[:, j, :], in_=ub[:, c * 128:(c + 1) * 128], identity=ident_b[:])
                        return ins
                    tr.op("pe", tp, reads=[uk, "ident_b"], writes=[pk])
                    st["n"] += 1
                    o = sg[:, g * 4:(g + 1) * 4, (eb % 4) * 128:(eb % 4 + 1) * 128]
                    tr.op("act", lambda e, o=o, pt=pt: e.copy(out=o, in_=pt[:, 0:4, :]), reads=[pk], writes=[sk])
                if eb % 4 == 3:
                    for hh in range(2):
                        tr.dma("act", ss, uT_d[(eb // 4) * 2 + hh], sg[:, :, hh * 256:(hh + 1) * 256],
                               reads=[sk], writes=["uT_d"])
            return block

        def phase_proj(own):
            xsrc = I("x_own") if own else I("x_pre")
            tag = "o" if own else "p"
            with ExitStack() as ph:
                sb, ps = mk_alloc(ph)
                xnT = sb("xnT" + tag, [128, 16, T], BF16)
                gB = sb("gB" + tag, [128, D], F32)
                s_g = tr.slot("gB" + tag)
                tr.dma("sp", s_g, gB[:], bcast_rows(I("norm_mix_g"), D), writes=["gB"])
                with ExitStack() as p0:
                    sb0, ps0 = mk_alloc(p0)
                    xr = Ring(tr, sb0, "x_ld" + tag, 2, [128, D], F32)
                    xnr = Ring(tr, sb0, "xn" + tag, 2, [128, D], BF16, slots=False)
                    junk = sb0("junk" + tag, [128, D], BF16)
                    ssr = Ring(tr, sb0, "ssq" + tag, 2, [128, 1], F32, slots=False)
                    rsr = Ring(tr, sb0, "rstd" + tag, 2, [128, 1], F32, slots=False)
                    ptr = Ring(tr, ps0, "x_pt" + tag, 2, [128, 8, 128], BF16, slots=False)
                    n = 0
                    for tt in range(16):
                        xb, xk, xs = xr.next()
                        tr.dma("sp", xs, xb[:], xsrc[tt * 128:(tt + 1) * 128, :], writes=[xk])
                        sq, sqk, _ = ssr.next()
                        rs, rsk, _ = rsr.next()
                        xn, xnk, _ = xnr.next()
                        tr.op("act", lambda e, xb=xb, sq=sq: e.activation(out=junk[:], in_=xb[:], func=AF.Square, accum_out=sq[:]),
                              reads=[xk], writes=["junk", sqk])
                        tr.op("act", lambda e, rs=rs, sq=sq: e.activation(out=rs[:], in_=sq[:], func=AF.Sqrt, scale=1.0 / D, bias=EPS),
                              reads=[sqk], writes=[rsk])
                        tr.op("dve", lambda e, rs=rs: e.reciprocal(out=rs[:], in_=rs[:]), reads=[rsk], writes=[rsk])
                        tr.op("dve", lambda e, xn=xn, xb=xb, rs=rs: e.scalar_tensor_tensor(
                            out=xn[:], in0=xb[:], scalar=rs[:, 0:1], in1=gB[:], op0=ALU.mult, op1=ALU.mult),
                            reads=[xk, rsk, "gB"], writes=[xnk])
                        for g in range(4):
                            pt, pk, _ = ptr.next()

                            def tp(e, g=g, xn=xn, pt=pt):
                                for j in range(4):
                                    c = g * 4 + j
                                    ins = e.transpose(out=pt[:, j, :], in_=xn[:, c * 128:(c + 1) * 128], identity=ident_b[:])
                                return ins
                            tr.op("pe", tp, reads=[xnk, "ident_b"], writes=[pk])
                            o = xnT[:, g * 4:(g + 1) * 4, tt * 128:(tt + 1) * 128]
                            if n % 2 == 0:
                                tr.op("dve", lambda e, o=o, pt=pt: e.tensor_copy(out=o, in_=pt[:, 0:4, :]), reads=[pk], writes=["xnT"])
                            else:
                                tr.op("act", lambda e, o=o, pt=pt: e.copy(out=o, in_=pt[:, 0:4, :]), reads=[pk], writes=["xnT"])
                            n += 1
                    tr.barrier()
                wr = Ring(tr, sb, "w_ld" + tag, 3, [128, 16, 256], BF16)
                pps = Ring(tr, ps, "pj_ps" + tag, 4, [128, 512], F32, slots=False)
                st16 = Ring(tr, sb, "pj_st" + tag, 3, [128, T], BF16)
                st32 = Ring(tr, sb, "pj_sf" + tag, 2, [128, T], F32)
                sig = Ring(tr, sb, "pj_sig" + tag, 2, [128, 512], F32, slots=False)
                vst = Ring(tr, sb, "pj_vs" + tag, 2, [128, 256], BF16)
                bg = sb("bg" + tag, [128, 32], F32)
                s_bg = tr.slot("bg" + tag)
                tr.dma("sp", s_bg, bg[:], I("b_gate"), writes=["bg"])
                w_view = I("w_in")
                cnt = [0]

                def load_w(col0):
                    wb, wk, ws = wr.next()
                    tr.dma("pool", ws, wb[:], w_view[col0 // 256], writes=[wk])
                    return wb, wk

                def fm_group(wb, wk, cb, tg, ntok=512, tok0=None):
                    pp, ppk, _ = pps.next()
                    t0 = tg * 512 if tok0 is None else tok0

                    def mm(e, pp=pp):
                        for c in range(16):
                            ins = e.matmul(pp[:, 0:ntok], lhsT=wb[:, c, cb * 128:(cb + 1) * 128],
                                           rhs=xnT[:, c, t0:t0 + ntok], start=(c == 0), stop=(c == 15))
                        return ins
                    tr.op("pe", mm, reads=[wk, "xnT"], writes=[ppk])
                    return pp, ppk

                def evac(pp, ppk, o, ok, scale=None):
                    cnt[0] += 1
                    if cnt[0] % 2 == 0:
                        if scale is None:
                            tr.op("dve", lambda e: e.tensor_copy(out=o, in_=pp[:, 0:o.shape[1]]), reads=[ppk], writes=[ok])
                        else:
                            tr.op("dve", lambda e: e.tensor_scalar(out=o, in0=pp[:, 0:o.shape[1]], scalar1=scale, scalar2=None,
                                                                  op0=ALU.mult), reads=[ppk], writes=[ok])
                    else:
                        if scale is None:
                            tr.op("act", lambda e: e.copy(out=o, in_=pp[:, 0:o.shape[1]]), reads=[ppk], writes=[ok])
                        else:
                            tr.op("act", lambda e: e.mul(out=o, in_=pp[:, 0:o.shape[1]], mul=scale), reads=[ppk], writes=[ok])

                plan = []
                if own:
                    plan += [("q", c0) for c0 in range(0, 1024, 256)]
                plan += [("k", 1024 + c0) for c0 in range(0, 1024, 256)]
                for kind, col0 in plan:
                    wb, wk = load_w(col0)
                    for cb in range(2):
                        stb, stk, sts = st16.next()
                        for tg in range(4):
                            pp, ppk = fm_group(wb, wk, cb, tg)
                            evac(pp, ppk, stb[:, tg * 512:(tg + 1) * 512], stk, scale=(0.125 if kind == "q" else None))
                        if kind == "q":
                            r0 = col0 + cb * 128
                            tr.dma("sp", sts, qT_d[r0:r0 + 128, :], stb[:], reads=[stk], writes=["qT_d"])
                        else:
                            r0 = col0 - 1024 + cb * 128
                            tcol = T if own else 0
                            tr.dma("sp", sts, kT_d[r0:r0 + 128, tcol:tcol + T], stb[:], reads=[stk], writes=["kT_d"])
                for c0 in range(0, 1024, 256):
                    wb, wk = load_w(2048 + c0)
                    for tt in range(16):
                        pp, ppk, _ = pps.next()

                        def mm(e, pp=pp, wb=wb, tt=tt):
                            for c in range(16):
                                ins = e.matmul(pp[:, 0:256], lhsT=xnT[:, c, tt * 128:(tt + 1) * 128], rhs=wb[:, c, :],
                                               start=(c == 0), stop=(c == 15))
                            return ins
                        tr.op("pe", mm, reads=[wk, "xnT"], writes=[ppk])
                        vb, vk, vs = vst.next()
                        evac(pp, ppk, vb[:], vk)
                        trow = (T if own else 0) + tt * 128
                        tr.dma("sp", vs, v_d[trow:trow + 128, c0:c0 + 256], vb[:], reads=[vk], writes=["v_d"])
                for c0 in range(0, 1024, 256):
                    wa, wak = load_w(3072 + c0)
                    wbb, wbk = load_w(4096 + c0)
                    for cb in range(2):
                        r0 = c0 + cb * 128
                        if own:
                            hb, hk, hs = st32.next()
                            for tg in range(4):
                                pa, pak = fm_group(wa, wak, cb, tg)
                                pb, pbk = fm_group(wbb, wbk, cb, tg)
                                sg, sgk, _ = sig.next()
                                tr.op("act", lambda e, sg=sg, pb=pb: e.activation(out=sg[:], in_=pb[:], func=AF.Sigmoid),
                                      reads=[pbk], writes=[sgk])
                                tr.op("dve", lambda e, hb=hb, pa=pa, sg=sg, tg=tg: e.tensor_tensor(
                                    out=hb[:, tg * 512:(tg + 1) * 512], in0=pa[:], in1=sg[:], op=ALU.mult),
                                    reads=[pak, sgk], writes=[hk])
                            tr.dma("sp", hs, hT_d[r0:r0 + 128, 32:32 + T], hb[:], reads=[hk], writes=["hT_d"])
                        else:
                            hb, hk, hs = st32.next()
                            pa, pak = fm_group(wa, wak, cb, 0, ntok=128, tok0=T - 128)
                            pb, pbk = fm_group(wbb, wbk, cb, 0, ntok=128, tok0=T - 128)
                            sg, sgk, _ = sig.next()
                            tr.op("act", lambda e, sg=sg, pb=pb: e.activation(out=sg[:, 0:128], in_=pb[:, 0:128], func=AF.Sigmoid),
                                  reads=[pbk], writes=[sgk])
                            tr.op("dve", lambda e, hb=hb, pa=pa, sg=sg: e.scalar_tensor_tensor(
                                out=hb[:, 0:128], in0=pa[:, 0:128], scalar=flags[:, 0:1], in1=sg[:, 0:128],
                                op0=ALU.mult, op1=ALU.mult), reads=[pak, sgk, "flags"], writes=[hk])
                            tr.dma("sp", hs, hT_d[r0:r0 + 128, 0:32], hb[:, 96:128], reads=[hk], writes=["hT_d"])
                if own:
                    for c0 in range(0, 4096, 256):
                        wb, wk = load_w(5120 + c0)
                        for cb in range(2):
                            blk = (c0 + cb * 128) // 128
                            stb, stk, sts = st16.next()
                            for tg in range(4):
                                pp, ppk = fm_group(wb, wk, cb, tg)
                                tr.op("act", lambda e, stb=stb, pp=pp, tg=tg, blk=blk: e.activation(
                                    out=stb[:, tg * 512:(tg + 1) * 512], in_=pp[:], func=AF.Sigmoid, bias=bg[:, blk:blk + 1]),
                                    reads=[ppk, "bg"], writes=[stk])
                            tr.dma("sp", sts, gates_d[blk * 128:(blk + 1) * 128, :], stb[:], reads=[stk], writes=["gates_d"])
                tr.barrier()


        def phase_attn(do_vcast=False, hook=None):
            with ExitStack() as ph:
                sb, ps = mk_alloc(ph)
                s_b = tr.slot("attn_setup")
                s_b2 = tr.slot("attn_setup2")
                lam_t = sb("lam_t", [128, 256], F32)
                lam4 = I("lam4")
                tr.dma("sp", s_b, lam_t[:], bass.AP(lam4.tensor, lam4.offset, [[0, 128], [1, 256]]), writes=["lam_t"])
                lp = sb("lam_p", [128, 128], F32)
                ls = sb("lam_s", [128, 2], F32)
                nlam = sb("nlam", [128, 1], F32)
                dummies = {en: sb("lam_dummy_" + en, [128, 512], F32) for en in ("dve", "act", "pool")}
                nl1 = sb("nl1", [128, 1], F32)
                ls2 = sb("lam_s2", [128, 2], F32)

                def settle():
                    tr.barrier()
                    for en in ("dve", "act", "pool"):
                        tr.op(en, lambda e, en=en: e.memset(dummies[en][:], 0.0) if en != "act" else e.memzero(dummies[en][:]), writes=["dummy_" + en])
                    tr.barrier()
                settle()
                tr.op("dve", lambda e: e.tensor_tensor(out=lp[:, 0:64], in0=lam_t[:, 0:64], in1=lam_t[:, 64:128], op=ALU.mult),
                      reads=["lam_t"], writes=["lp"])
                tr.op("dve", lambda e: e.tensor_tensor(out=lp[:, 64:128], in0=lam_t[:, 128:192], in1=lam_t[:, 192:256], op=ALU.mult),
                      reads=["lam_t"], writes=["lp"])
                settle()
                tr.op("dve", lambda e: e.reduce_sum(out=ls[:, 0:1], in_=lp[:, 0:64], axis=AX.X), reads=["lp"], writes=["ls"])
                tr.op("dve", lambda e: e.reduce_sum(out=ls[:, 1:2], in_=lp[:, 64:128], axis=AX.X), reads=["lp"], writes=["ls"])
                settle()
                tr.op("act", lambda e: e.activation(out=ls2[:], in_=ls[:], func=AF.Exp), reads=["ls"], writes=["ls2"])
                settle()
                tr.op("dve", lambda e: e.tensor_tensor(out=nl1[:], in0=ls2[:, 1:2], in1=ls2[:, 0:1], op=ALU.subtract),
                      reads=["ls2"], writes=["nl1"])
                settle()
                tr.op("dve", lambda e: e.tensor_scalar(out=nlam[:], in0=nl1[:], scalar1=-LAM_INIT, scalar2=None, op0=ALU.add),
                      reads=["nl1"], writes=["nlam"])
                settle()
                rb = sb("rb", [32, 8], F32)
                ohs = sb("ohs", [32, 512], F32)
                grs = sb("grs", [8, 512], F32)
                tr.dma("sp", tr.slot("attn_rb"), rb[:], I("rel_bias"), writes=["rb"])
                tr.dma("sp", tr.slot("attn_oh"), ohs[:], I("oh_r"), writes=["ohs"])
                with ExitStack() as p1:
                    _, ps1 = mk_alloc(p1)
                    gps_ = ps1("gr_ps", [8, 512], F32)
                    tr.op("pe", lambda e: e.matmul(gps_[:], lhsT=rb[:], rhs=ohs[:], start=True, stop=True),
                          reads=["rb", "ohs"], writes=["gr_ps"])
                    tr.op("dve", lambda e: e.tensor_copy(out=grs[:], in_=gps_[:]), reads=["gr_ps"], writes=["grs"])
                    tr.dma("sp", s_b2, gr_d, grs[:], reads=["grs"], writes=["gr_d"])
                    tr.barrier()
                hank = sb("hank", [128, 8, 2, 128], F32)
                for h in range(8):
                    tr.dma("sp", s_b, hank[:, h, 0, :], bass.AP(gr_d.tensor, h * 512 + 129, [[1, 128], [1, 128]]),
                           reads=["gr_d"], writes=["hank"])
                    tr.dma("sp", s_b, hank[:, h, 1, :], bass.AP(gr_d.tensor, h * 512 + 257, [[1, 128], [1, 128]]),
                           reads=["gr_d"], writes=["hank"])
                cB = sb("cB", [128, 8], F32)
                cBm = sb("cBm", [128, 8], F32)
                relb = I("rel_bias")
                tr.dma("sp", s_b, cB[:], bass.AP(relb.tensor, relb.offset + 15 * 8, [[0, 128], [1, 8]]), writes=["cB"])
                sgB = sb("sgB", [128, 128], F32)
                tr.dma("sp", s_b, sgB[:], bcast_rows(I("subln_g"), 128), writes=["sgB"])
                tr.barrier()
                tr.op("dve", lambda e: e.tensor_scalar(out=sgB[:], in0=sgB[:], scalar1=1.0 - LAM_INIT, scalar2=None, op0=ALU.mult),
                      reads=["sgB"], writes=["sgB"])
                tr.op("dve", lambda e: e.tensor_scalar(out=cBm[:], in0=cB[:], scalar1=flags[:, 1:2], scalar2=None, op0=ALU.add),
                      reads=["cB", "flags"], writes=["cBm"])
                tiles = sb("btiles", [128, 8, 5, 128], BF16)
                tr.op("dve", lambda e: e.tensor_copy(out=tiles[:, :, 0, :], in_=hank[:, :, 0, :]), reads=["hank"], writes=["tiles"])
                tr.op("dve", lambda e: e.tensor_copy(out=tiles[:, :, 1, :], in_=hank[:, :, 1, :]), reads=["hank"], writes=["tiles"])
                tr.op("dve", lambda e: e.tensor_copy(out=tiles[:, :, 2, :], in_=cB[:].unsqueeze(2).to_broadcast([128, 8, 128])),
                      reads=["cB"], writes=["tiles"])
                tr.op("dve", lambda e: e.tensor_scalar(out=tiles[:, :, 3, :], in0=hank[:, :, 1, :], scalar1=flags[:, 1:2], scalar2=None,
                                                      op0=ALU.add), reads=["hank", "flags"], writes=["tiles"])
                tr.op("dve", lambda e: e.tensor_copy(out=tiles[:, :, 4, :], in_=cBm[:].unsqueeze(2).to_broadcast([128, 8, 128])),
                      reads=["cBm"], writes=["tiles"])
                tr.barrier()
                tr.op("dve", lambda e: e.memset(tiles[0:64, :, 0, 0:64], NEG), writes=["tiles"])
                tr.barrier()

                kr = Ring(tr, sb, "kT_h", 2, [128, 2 * T], BF16)
                qr = Ring(tr, sb, "qT_h", 2, [128, T], BF16)
                vr = Ring(tr, sb, "v_h", 2, [128, 32, 129], BF16)
                for vb_ in vr.bufs:
                    tr.op("pool", lambda e, vb_=vb_: e.memset(vb_[:, :, 128:129], 1.0), writes=["vones"])
                tr.barrier()
                sring = Ring(tr, ps, "s_ps", 2, [128, 2, 512], F32, slots=False)
                oA = [ps("oA0", [128, 512], F32), ps("oA1", [128, 512], F32)]
                oC = ps("oC", [128, 512], F32)
                tps = ps("o_tp", [128, 128], BF16)
                ptr_ = Ring(tr, sb, "pT", 3, [128, 2, 512], BF16, slots=False)
                ostg = Ring(tr, sb, "oT_st", 2, [128, T], BF16)
                rz = sb("rz", [128, 4], F32)
                rzA = sb("rzA", [128, 1], F32)
                t0 = sb("o_t0", [128, 128], F32)
                ob = sb("o_ob", [128, 128], F32)
                ojunk = sb("o_junk", [128, 128], F32)
                onb = sb("o_onb", [128, 128], BF16)

                if "dbgO" in debug:
                    dbgO_s = sb("dbgO_s", [128, 3, 512], F32)

                def oreg(m, j):
                    if j < 3:
                        return oA[m][:, j * 160:j * 160 + 129]
                    return oC[:, m * 256:m * 256 + 129]

                for h in range(8):
                    if do_vcast:
                        vcast_piece(2 * h)
                        vcast_piece(2 * h + 1)
                    kb_, kk, ks = kr.next()
                    qb_, qk_, qs = qr.next()
                    vb_, vk, vs = vr.next()
                    tr.dma("sp", ks, kb_[:], kT_d[h * 128:(h + 1) * 128, :], writes=[kk])
                    tr.dma("sp", qs, qb_[:], qT_d[h * 128:(h + 1) * 128, :], writes=[qk_])
                    tr.dma("sp", vs, vb_[:, :, 0:128], v_d[:, h * 128:(h + 1) * 128].rearrange("(kb p) e -> p kb e", p=128),
                           writes=[vk])
                    osb, osk, oss = ostg.next()
                    for g in range(4):
                        Q0 = 16 + 4 * g
                        nkb = Q0 + 4

                        def qk(kb, g=g, Q0=Q0, h=h, kb_=kb_, qb_=qb_, kk=kk, qk_=qk_):
                            jlo = max(0, kb - Q0)
                            mixed = kb >= Q0 - 1
                            S, Sk, _ = sring.next()

                            def mm(e):
                                for m in range(2):
                                    ins = e.matmul(S[:, m, jlo * 128:512], lhsT=kb_[m * 64:(m + 1) * 64, kb * 128:(kb + 1) * 128],
                                                   rhs=qb_[m * 64:(m + 1) * 64, g * 512 + jlo * 128:(g + 1) * 512],
                                                   start=True, stop=not mixed)
                                if mixed:
                                    for j in range(jlo, 4):
                                        if kb == Q0 + j:
                                            kind = 0
                                        elif kb == Q0 + j - 1:
                                            kind = 3 if kb < 16 else 1
                                        else:
                                            kind = 4 if kb < 16 else 2
                                        for m in range(2):
                                            ins = e.matmul(S[:, m, j * 128:(j + 1) * 128], lhsT=anti_b[:], rhs=tiles[:, h, kind, :],
                                                           start=False, stop=(j == 3))
                                return ins
                            tr.op("pe", mm, reads=[kk, qk_], writes=[Sk])
                            P, Pk, _ = ptr_.next()
                            if mixed:
                                tr.op("act", lambda e: e.activation(out=P[:, :, jlo * 128:512], in_=S[:, :, jlo * 128:512], func=AF.Exp),
                                      reads=[Sk], writes=[Pk])
                            else:
                                bias = (cB if kb >= 16 else cBm)[:, h:h + 1]
                                tr.op("act", lambda e: e.activation(out=P[:], in_=S[:], func=AF.Exp, bias=bias),
                                      reads=[Sk], writes=[Pk])
                            if "dbgP" in debug and h == 0 and g == 0 and kb == 3:
                                tr.dma("sp", s_b2, dbgP, P[:], reads=[Pk], writes=["dbgP"])
                            return P, Pk, jlo

                        def pv(kb, P, Pk, jlo, Q0=Q0, vb_=vb_, vk=vk):
                            def mm(e):
                                for j in range(jlo, 4):
                                    for m in range(2):
                                        ins = e.matmul(oreg(m, j), lhsT=P[:, m, j * 128:(j + 1) * 128], rhs=vb_[:, kb, :],
                                                       start=(kb == 0 and (j == 0 or (j == 3 and m == 0))),
                                                       stop=((j == 2 and kb == Q0 + 2) or (j == 3 and m == 1 and kb == Q0 + 3)))
                                return ins
                            tr.op("pe", mm, reads=[Pk, vk], writes=["oacc"])

                        pend = None
                        for kb in range(nkb):
                            cur = qk(kb)
                            if pend is not None:
                                pv(*pend)
                            pend = (kb,) + cur
                        pv(*pend)
                        if hook is not None:
                            hook()
                        if "dbgO" in debug and h == 0 and g == 0:
                            for ii, src in enumerate((oA[0], oA[1], oC)):
                                tr.op("dve", lambda e, ii=ii, src=src: e.tensor_copy(out=dbgO_s[:, ii, :], in_=src[:]), reads=["oacc"], writes=["dbgO_s"])
                            tr.dma("sp", s_b2, dbgO, dbgO_s[:], reads=["dbgO_s"], writes=["dbgO"])
                        for j in range(4):
                            r0, r1 = oreg(0, j), oreg(1, j)
                            tr.op("dve", lambda e: e.reciprocal(out=rz[:, 0:1], in_=r0[:, 128:129]), reads=["oacc"], writes=["rz"])
                            tr.op("dve", lambda e: e.reciprocal(out=rz[:, 1:2], in_=r1[:, 128:129]), reads=["oacc"], writes=["rz"])
                            tr.op("dve", lambda e: e.tensor_scalar(out=t0[:], in0=r0[:, 0:128], scalar1=rz[:, 0:1], scalar2=None, op0=ALU.mult),
                                  reads=["oacc", "rz"], writes=["t0"])
                            tr.op("dve", lambda e: e.tensor_tensor(out=rz[:, 2:3], in0=rz[:, 1:2], in1=nlam[:], op=ALU.mult),
                                  reads=["rz"], writes=["rz2"])
                            tr.op("dve", lambda e: e.scalar_tensor_tensor(out=ob[:], in0=r1[:, 0:128], scalar=rz[:, 2:3], in1=t0[:],
                                                                         op0=ALU.mult, op1=ALU.add),
                                  reads=["oacc", "rz2", "t0"], writes=["ob"])
                            tr.op("act", lambda e: e.activation(out=ojunk[:], in_=ob[:], func=AF.Square, accum_out=rzA[:]),
                                  reads=["ob"], writes=["ojunk", "rz3"])
                            tr.op("act", lambda e: e.activation(out=rzA[:], in_=rzA[:], func=AF.Sqrt, scale=1.0 / 128, bias=EPS),
                                  reads=["rz3"], writes=["rz3"])
                            tr.op("dve", lambda e: e.reciprocal(out=rzA[:], in_=rzA[:]), reads=["rz3"], writes=["rz3"])
                            tr.op("dve", lambda e: e.scalar_tensor_tensor(out=onb[:], in0=ob[:], scalar=rzA[:, 0:1], in1=sgB[:],
                                                                         op0=ALU.mult, op1=ALU.mult),
                                  reads=["ob", "rz3"], writes=["onb"])
                            tr.op("pe", lambda e: e.transpose(out=tps[:], in_=onb[:], identity=ident_b[:]), reads=["onb"], writes=["tps"])
                            if "dbgE" in debug and h == 0 and g == 0 and j == 0:
                                tr.dma("sp", s_b2, dbgE[:, 0:128], ob[:], reads=["ob"], writes=["dbgE"])
                                tr.dma("sp", s_b2, dbgE[:, 128:132], rz[:], reads=["rz", "rz2"], writes=["dbgE"])
                                tr.dma("sp", s_b2, dbgE[:, 132:260], t0[:], reads=["t0"], writes=["dbgE"])
                                tr.barrier()
                            c0 = (4 * g + j) * 128
                            tr.op("dve", lambda e, c0=c0: e.tensor_copy(out=osb[:, c0:c0 + 128], in_=tps[:]), reads=["tps"], writes=[osk])
                    tr.dma("act", oss, oT_d[h * 128:(h + 1) * 128, :], osb[:], reads=[osk], writes=["oT_d"])
                tr.barrier()


        def conv_pre(stack):
            sbc, _ = mk_alloc(stack)
            s_c1 = tr.slot("conv_setup")
            cw = sbc("cw", [128, 8, 31], F32)
            cvec = sbc("cvec", [128, 3, 8], F32)
            tr.dma("sp", s_c1, cw[:], I("conv_w"), writes=["cw"])
            tr.dma("sp", tr.slot("conv_setup2"), cvec[:], I("cvec_in"), writes=["cvec"])
            cv = sbc("convo", [128, 8, T], F32)
            hr = Ring(tr, sbc, "h_ld", 2, [128, 32 + T], F32)
            ops = []
            cur = {}

            def mk(k, j):
                def f():
                    if j == 0:
                        hb, hk, hs = hr.next()
                        tr.dma("sp", hs, hb[:], hT_d[k * 128:(k + 1) * 128, :], writes=[hk])
                        cur["h"] = (hb, hk)
                    hb, hk = cur["h"]
                    src = hb[:, 2 + j:2 + j + T]
                    if j == 0:
                        tr.op("dve", lambda e: e.tensor_scalar(
                            out=cv[:, k, :], in0=src, scalar1=cw[:, k, j:j + 1], scalar2=cvec[:, 0, k:k + 1],
                            op0=ALU.mult, op1=ALU.add), reads=[hk, "cw", "cvec"], writes=["cv%d" % k])
                    else:
                        tr.op("dve", lambda e: e.scalar_tensor_tensor(
                            out=cv[:, k, :], in0=src, scalar=cw[:, k, j:j + 1], in1=cv[:, k, :],
                            op0=ALU.mult, op1=ALU.add), reads=[hk, "cw", "cv%d" % k], writes=["cv%d" % k])
                return f
            for k in range(8):
                for j in range(31):
                    ops.append(mk(k, j))
            return {"cw": cw, "cvec": cvec, "cv": cv, "ops": ops}

        def conv_hook(cst, n=8):
            for _ in range(n):
                if cst["ops"]:
                    cst["ops"].pop(0)()

        def phase_conv_mix(cst):
            with ExitStack() as ph:
                sb, ps = mk_alloc(ph)
                sT = sb("sT", [128, 8, T], BF16)
                with ExitStack() as pc:
                    sbc, psc = mk_alloc(pc)
                    cw, cvec, cv = cst["cw"], cst["cvec"], cst["cv"]
                    while cst["ops"]:
                        cst["ops"].pop(0)()
                    sqr = Ring(tr, sbc, "c_sq", 2, [128, T], F32, slots=False)
                    mean_ps = psc("mean_ps", [128, 4, 512], F32)
                    ex2_ps = psc("ex2_ps", [128, 4, 512], F32)
                    for k in range(8):
                        sq, sqk, _ = sqr.next()
                        tr.op("act", lambda e, sq=sq, k=k: e.activation(out=sq[:], in_=cv[:, k, :], func=AF.Square),
                              reads=["cv%d" % k], writes=[sqk])

                        def st(e, k=k, sq=sq):
                            for tg in range(4):
                                e.matmul(mean_ps[:, tg, :], lhsT=ones_f[:], rhs=cv[:, k, tg * 512:(tg + 1) * 512],
                                         start=(k == 0), stop=(k == 7))
                                ins = e.matmul(ex2_ps[:, tg, :], lhsT=ones_f[:], rhs=sq[:, tg * 512:(tg + 1) * 512],
                                               start=(k == 0), stop=(k == 7))
                            return ins
                        tr.op("pe", st, reads=["cv%d" % k, sqk, "ones_f"], writes=["stats_ps"])
                    mean = sbc("c_mean", [128, T], F32)
                    rstd = sbc("c_rstd", [128, T], F32)
                    tr.op("act", lambda e: e.mul(out=mean[:], in_=mean_ps[:].rearrange("p a b -> p (a b)"), mul=1.0 / 1024),
                          reads=["stats_ps"], writes=["mean"])
                    tr.op("dve", lambda e: e.tensor_tensor(out=rstd[:], in0=mean[:], in1=mean[:], op=ALU.mult),
                          reads=["mean"], writes=["rstd"])
                    tr.op("dve", lambda e: e.scalar_tensor_tensor(out=rstd[:], in0=ex2_ps[:].rearrange("p a b -> p (a b)"),
                                                                 scalar=1.0 / 1024, in1=rstd[:], op0=ALU.mult, op1=ALU.subtract),
                          reads=["stats_ps", "rstd"], writes=["rstd"])
                    tr.op("act", lambda e: e.activation(out=rstd[:], in_=rstd[:], func=AF.Sqrt, bias=EPS),
                          reads=["rstd"], writes=["rstd"])
                    tr.op("dve", lambda e: e.reciprocal(out=rstd[:], in_=rstd[:]), reads=["rstd"], writes=["rstd"])
                    for k in range(8):
                        tr.op("dve", lambda e, k=k: e.tensor_tensor(out=cv[:, k, :], in0=cv[:, k, :], in1=mean[:], op=ALU.subtract),
                              reads=["cv%d" % k, "mean"], writes=["cv%d" % k])
                        tr.op("pool", lambda e, k=k: e.tensor_tensor(out=cv[:, k, :], in0=cv[:, k, :], in1=rstd[:], op=ALU.mult),
                              reads=["cv%d" % k, "rstd"], writes=["cv%d" % k])
                        tr.op("act", lambda e, k=k: e.activation(out=sT[:, k, :], in_=cv[:, k, :], func=AF.Silu,
                                                                scale=cvec[:, 1, k:k + 1], bias=cvec[:, 2, k:k + 1]),
                              reads=["cv%d" % k, "cvec"], writes=["sT"])
                    tr.barrier()
                oT = sb("oT_s", [128, 8, T], BF16)
                s_o = tr.slot("oT_ld")
                tr.dma("sp", s_o, oT[:], oT_d.rearrange("(k p) t -> p k t", p=128), writes=["oT"])
                war = Ring(tr, sb, "wao", 2, [128, 8, 256], BF16)
                wcr = Ring(tr, sb, "wco", 2, [128, 8, 256], BF16)
                gar = Ring(tr, sb, "g_a", 2, [128, T], BF16)
                gcr = Ring(tr, sb, "g_c", 2, [128, T], BF16)
                mst = Ring(tr, sb, "mix_st", 2, [128, T], BF16)
                t1r = Ring(tr, sb, "mix_t1", 2, [128, 512], F32, slots=False)
                t2r = Ring(tr, sb, "mix_t2", 2, [128, 512], F32, slots=False)
                pa = Ring(tr, ps, "ya_ps", 2, [128, 512], F32, slots=False)
                pc_ = Ring(tr, ps, "yc_ps", 2, [128, 512], F32, slots=False)
                wao_v = I("w_att_out")
                wco_v = I("w_conv_out")
                for f2 in range(8):
                    wa, wak, was = war.next()
                    wc, wck, wcs = wcr.next()
                    tr.dma("pool", was, wa[:], wao_v[f2], writes=[wak])
                    tr.dma("pool", wcs, wc[:], wco_v[f2], writes=[wck])
                    for fb in range(2):
                        f = f2 * 2 + fb
                        ga, gak, gas = gar.next()
                        gc, gck, gcs = gcr.next()
                        tr.dma("sp", gas, ga[:], gates_d[f * 128:(f + 1) * 128, :], writes=[gak])
                        tr.dma("sp", gcs, gc[:], gates_d[2048 + f * 128:2048 + (f + 1) * 128, :], writes=[gck])
                        mb, mk, ms = mst.next()
                        for tg in range(4):
                            pA, pAk, _ = pa.next()
                            pC, pCk, _ = pc_.next()

                            def mm(e, pA=pA, pC=pC, wa=wa, wc=wc, fb=fb, tg=tg):
                                for k in range(8):
                                    e.matmul(pA[:], lhsT=wa[:, k, fb * 128:(fb + 1) * 128], rhs=oT[:, k, tg * 512:(tg + 1) * 512],
                                             start=(k == 0), stop=(k == 7))
                                for k in range(8):
                                    ins = e.matmul(pC[:], lhsT=wc[:, k, fb * 128:(fb + 1) * 128], rhs=sT[:, k, tg * 512:(tg + 1) * 512],
                                                   start=(k == 0), stop=(k == 7))
                                return ins
                            tr.op("pe", mm, reads=[wak, wck, "oT", "sT"], writes=[pAk, pCk])
                            t1, t1k, _ = t1r.next()
                            t2, t2k, _ = t2r.next()
                            tsl = slice(tg * 512, (tg + 1) * 512)
                            tr.op("dve", lambda e, t1=t1, pA=pA, ga=ga, tsl=tsl: e.tensor_tensor(out=t1[:], in0=pA[:], in1=ga[:, tsl], op=ALU.mult),
                                  reads=[pAk, gak], writes=[t1k])
                            tr.op("dve", lambda e, t2=t2, pC=pC, gc=gc, tsl=tsl: e.tensor_tensor(out=t2[:], in0=pC[:], in1=gc[:, tsl], op=ALU.mult),
                                  reads=[pCk, gck], writes=[t2k])
                            tr.op("pool", lambda e, mb=mb, t1=t1, t2=t2, tsl=tsl: e.tensor_tensor(out=mb[:, tsl], in0=t1[:], in1=t2[:], op=ALU.add),
                                  reads=[t1k, t2k], writes=[mk])
                        tr.dma("act", ms, mixT_d[f * 128:(f + 1) * 128, :], mb[:], reads=[mk], writes=["mixT_d"])
                tr.barrier()

        def phase_wout():
            with ExitStack() as ph:
                sb, ps = mk_alloc(ph)
                wo = sb("wo_s", [128, 16, D], BF16)
                s_w = tr.slot("wo_ld")
                wo_v = I("w_out").rearrange("(c p) n -> p c n", p=128)
                for c4 in range(4):
                    tr.dma("pool", s_w, wo[:, c4 * 4:(c4 + 1) * 4, :], wo_v[:, c4 * 4:(c4 + 1) * 4, :], writes=["wo"])
                gB = sb("gB2", [128, D], F32)
                tr.dma("sp", tr.slot("gB2_ld"), gB[:], bcast_rows(I("norm_ffn_g"), D), writes=["gB2"])
                tr.barrier()
                mr = Ring(tr, sb, "mixT_ld", 2, [128, 16, 512], BF16)
                xr = Ring(tr, sb, "x2_ld", 2, [128, D], F32)
                hr = Ring(tr, sb, "h1_st", 2, [128, D], F32)
                xnr = Ring(tr, sb, "xn2", 2, [128, D], BF16, slots=False)
                junk = sb("junk2", [128, D], BF16)
                ssr = Ring(tr, sb, "ssq2", 2, [128, 1], F32, slots=False)
                xts = Ring(tr, sb, "xn2T_st", 2, [128, 16, 512], BF16)
                pps = Ring(tr, ps, "wo_ps", 4, [128, 512], F32, slots=False)
                ptr = Ring(tr, ps, "x2_pt", 2, [128, 8, 128], BF16, slots=False)
                mix_v = mixT_d.rearrange("(c p) t -> p c t", p=128)
                n = 0
                for tg in range(4):
                    mb, mk, ms = mr.next()
                    tr.dma("sp", ms, mb[:], mix_v[:, :, tg * 512:(tg + 1) * 512], writes=[mk])
                    xt, xtk, xtsl = xts.next()
                    for t4 in range(4):
                        tt = tg * 4 + t4
                        xb, xk, xs = xr.next()
                        tr.dma("sp", xs, xb[:], I("x_own")[tt * 128:(tt + 1) * 128, :], writes=[xk])
                        hb, hk, hs = hr.next()
                        for dc in range(4):
                            pp, ppk, _ = pps.next()

                            def mm(e, pp=pp, mb=mb, t4=t4, dc=dc):
                                for c in range(16):
                                    ins = e.matmul(pp[:], lhsT=mb[:, c, t4 * 128:(t4 + 1) * 128], rhs=wo[:, c, dc * 512:(dc + 1) * 512],
                                                   start=(c == 0), stop=(c == 15))
                                return ins
                            tr.op("pe", mm, reads=[mk, "wo"], writes=[ppk])
                            tr.op("dve", lambda e, hb=hb, pp=pp, xb=xb, dc=dc: e.tensor_tensor(
                                out=hb[:, dc * 512:(dc + 1) * 512], in0=pp[:], in1=xb[:, dc * 512:(dc + 1) * 512], op=ALU.add),
                                reads=[ppk, xk], writes=[hk])
                        tr.dma("act", hs, h1_d[tt * 128:(tt + 1) * 128, :], hb[:], reads=[hk], writes=["h1_d"])
                        sq, sqk, _ = ssr.next()
                        xn, xnk, _ = xnr.next()
                        tr.op("act", lambda e, hb=hb, sq=sq: e.activation(out=junk[:], in_=hb[:], func=AF.Square, accum_out=sq[:]),
                              reads=[hk], writes=["junk2", sqk])
                        tr.op("act", lambda e, sq=sq: e.activation(out=sq[:], in_=sq[:], func=AF.Sqrt, scale=1.0 / D, bias=EPS),
                              reads=[sqk], writes=[sqk])
                        tr.op("dve", lambda e, sq=sq: e.reciprocal(out=sq[:], in_=sq[:]), reads=[sqk], writes=[sqk])
                        tr.op("dve", lambda e, xn=xn, hb=hb, sq=sq: e.scalar_tensor_tensor(
                            out=xn[:], in0=hb[:], scalar=sq[:, 0:1], in1=gB[:], op0=ALU.mult, op1=ALU.mult),
                            reads=[hk, sqk, "gB2"], writes=[xnk])
                        for g in range(4):
                            pt, pk, _ = ptr.next()

                            def tp(e, g=g, xn=xn, pt=pt):
                                for j in range(4):
                                    c = g * 4 + j
                                    ins = e.transpose(out=pt[:, j, :], in_=xn[:, c * 128:(c + 1) * 128], identity=ident_b[:])
                                return ins
                            tr.op("pe", tp, reads=[xnk], writes=[pk])
                            o = xt[:, g * 4:(g + 1) * 4, t4 * 128:(t4 + 1) * 128]
                            if n % 2 == 0:
                                tr.op("dve", lambda e, o=o, pt=pt: e.tensor_copy(out=o, in_=pt[:, 0:4, :]), reads=[pk], writes=[xtk])
                            else:
                                tr.op("act", lambda e, o=o, pt=pt: e.copy(out=o, in_=pt[:, 0:4, :]), reads=[pk], writes=[xtk])
                            n += 1
                    tr.dma("act", xtsl, xn2T_d[:, :, tg * 512:(tg + 1) * 512].rearrange("c p t -> p c t"), xt[:],
                           reads=[xtk], writes=["xn2T_d"])
                tr.barrier()


        sc_d = scratch("sc_d", [T, 2048], F32)
        tb_d = scratch("tb_d", [T, 16], F32)

        def phase_route():
            with ExitStack() as ph:
                sb, ps = mk_alloc(ph)
                wq = sb("wq_s", [128, 16, D], BF16)
                s_w = tr.slot("wq_ld")
                wq_v = I("peer_w_q").rearrange("(c p) n -> p c n", p=128)
                for c4 in range(4):
                    tr.dma("pool", s_w, wq[:, c4 * 4:(c4 + 1) * 4, :], wq_v[:, c4 * 4:(c4 + 1) * 4, :], writes=["wq"])
                skn = sb("skn", [128, 16, 128], BF16)
                skT = sb("skT", [128, 16, 128], BF16)
                tr.dma("pool", s_w, skn[:], I("peer_sub_keys").rearrange("b n c -> n b c"), writes=["skn"])
                tr.barrier()
                ptr = Ring(tr, ps, "sk_pt", 2, [128, 8, 128], BF16, slots=False)
                for g in range(4):
                    pt, pk, _ = ptr.next()

                    def tp(e, g=g, pt=pt):
                        for j in range(4):
                            ins = e.transpose(out=pt[:, j, :], in_=skn[:, g * 4 + j, :], identity=ident_b[:])
                        return ins
                    tr.op("pe", tp, reads=["skn"], writes=[pk])
                    tr.op("dve", lambda e, g=g, pt=pt: e.tensor_copy(out=skT[:, g * 4:(g + 1) * 4, :], in_=pt[:, 0:4, :]), reads=[pk], writes=["skT"])
                tr.barrier()
                xr = Ring(tr, sb, "xt_ld", 2, [128, 16, 128], BF16)
                qps = Ring(tr, ps, "q_ps", 2, [128, 4, 128], F32, slots=False)
                sps = Ring(tr, ps, "sc_ps", 2, [128, 4, 128], F32, slots=False)
                qTp = sb("qTp", [128, 16, 128], BF16)
                scr = Ring(tr, sb, "sc_s", 2, [128, 16, 128], F32)
                sc2 = sb("sc2", [128, 16, 128], F32)
                mx = sb("mx", [128, 16], F32)
                etr = Ring(tr, sb, "et_s", 2, [128, 16, 128], F32)
                top = sb("top", [128, 16, 16], F32)
                cand = sb("cand", [128, 8, 256], F32)
                cand2 = sb("cand2", [128, 8, 256], F32)
                cvv = sb("cvv", [128, 8, 16], F32)
                ee = sb("ee", [128, 8, 16], F32)
                zz = sb("zz", [128, 8], F32)
                tbr = Ring(tr, sb, "tb_s", 2, [128, 16], F32)
                xn2T_v = xn2T_d.rearrange("c p t -> p c t")
                tblock = make_table_emitter(sb, ps)
                pending_stores = []
                for tt in range(16):
                    for eb in range(tt * 8, tt * 8 + 8):
                        tblock(eb)
                    while pending_stores:
                        pending_stores.pop(0)()
                    xt, xk, xs = xr.next()
                    tr.dma("sp", xs, xt[:], xn2T_v[:, :, tt * 128:(tt + 1) * 128], writes=[xk])
                    for g in range(4):
                        qp, qpk, _ = qps.next()

                        def mm(e, g=g, qp=qp, xt=xt):
                            for j in range(4):
                                blk = g * 4 + j
                                for c in range(16):
                                    ins = e.matmul(qp[:, j, :], lhsT=wq[:, c, blk * 128:(blk + 1) * 128], rhs=xt[:, c, :],
                                                   start=(c == 0), stop=(c == 15))
                            return ins
                        tr.op("pe", mm, reads=["wq", xk], writes=[qpk])
                        tr.op("act", lambda e, g=g, qp=qp: e.copy(out=qTp[:, g * 4:(g + 1) * 4, :], in_=qp[:]), reads=[qpk], writes=["qTp%d" % g])
                    sc, sck, scs = scr.next()
                    for g in range(4):
                        sp_, spk, _ = sps.next()

                        def mm2(e, g=g, sp_=sp_):
                            for j in range(4):
                                blk = g * 4 + j
                                ins = e.matmul(sp_[:, j, :], lhsT=qTp[:, blk, :], rhs=skT[:, blk, :], start=True, stop=True)
                            return ins
                        tr.op("pe", mm2, reads=["qTp%d" % g, "skT"], writes=[spk])
                        tr.op("act", lambda e, g=g, sp_=sp_, sc=sc: e.copy(out=sc[:, g * 4:(g + 1) * 4, :], in_=sp_[:]), reads=[spk], writes=[sck])
                    tr.op("dve", lambda e, sc=sc: e.tensor_reduce(out=mx[:], in_=sc[:], axis=AX.X, op=ALU.max), reads=[sck], writes=["mx"])
                    tr.op("dve", lambda e, sc=sc: e.tensor_tensor(out=sc2[:], in0=sc[:], in1=mx[:].unsqueeze(2).to_broadcast([128, 16, 128]),
                                                                 op=ALU.subtract), reads=[sck, "mx"], writes=["sc2"])
                    et, etk, ets = etr.next()
                    tr.op("act", lambda e, et=et: e.activation(out=et[:], in_=sc2[:], func=AF.Exp), reads=["sc2"], writes=[etk])
                    for blk in range(16):
                        tr.op("dve", lambda e, blk=blk, et=et: e.max(out=top[:, blk, 0:8], in_=et[:, blk, :]), reads=[etk], writes=["top"])
                        tr.op("dve", lambda e, blk=blk, et=et: e.match_replace(out=sc2[:, blk, :], in_to_replace=top[:, blk, 0:8],
                                                                             in_values=et[:, blk, :], imm_value=-1.0),
                              reads=[etk, "top"], writes=["sc2"])
                        tr.op("dve", lambda e, blk=blk: e.max(out=top[:, blk, 8:16], in_=sc2[:, blk, :]), reads=["sc2"], writes=["top"])
                    topv = top[:].rearrange("p (h two) k -> p h two k", two=2)
                    in0 = topv[:, :, 0, :].unsqueeze(3).to_broadcast([128, 8, 16, 16])
                    in1 = topv[:, :, 1, :].unsqueeze(2).to_broadcast([128, 8, 16, 16])
                    tr.op("dve", lambda e, in0=in0, in1=in1: e.tensor_tensor(
                        out=cand[:].rearrange("p h (a b) -> p h a b", a=16), in0=in0, in1=in1, op=ALU.mult),
                        reads=["top"], writes=["cand"])
                    for h in range(8):
                        tr.op("dve", lambda e, h=h: e.max(out=cvv[:, h, 0:8], in_=cand[:, h, :]), reads=["cand"], writes=["cvv"])
                        tr.op("dve", lambda e, h=h: e.match_replace(out=cand2[:, h, :], in_to_replace=cvv[:, h, 0:8],
                                                                   in_values=cand[:, h, :], imm_value=-1.0),
                              reads=["cand", "cvv"], writes=["cand2"])
                        tr.op("dve", lambda e, h=h: e.max(out=cvv[:, h, 8:16], in_=cand2[:, h, :]), reads=["cand2"], writes=["cvv"])
                    tb, tbk, tbs = tbr.next()
                    tr.op("dve", lambda e: e.reduce_sum(out=zz[:], in_=cvv[:], axis=AX.X), reads=["cvv"], writes=["zz"])
                    tr.op("dve", lambda e, tb=tb: e.reciprocal(out=tb[:, 8:16], in_=zz[:]), reads=["zz"], writes=[tbk])
                    etv = et[:].rearrange("p (h two) n -> p h two n", two=2)
                    tr.op("dve", lambda e, tb=tb, etv=etv: e.tensor_tensor(out=etv[:, :, 0, :], in0=etv[:, :, 0, :],
                                                                       in1=tb[:, 8:16].unsqueeze(2).to_broadcast([128, 8, 128]), op=ALU.mult),
                          reads=[etk, tbk], writes=[etk])
                    tr.op("dve", lambda e, tb=tb: e.tensor_tensor(out=topv[:, :, 0, :], in0=topv[:, :, 0, :],
                                                                 in1=tb[:, 8:16].unsqueeze(2).to_broadcast([128, 8, 16]), op=ALU.mult),
                          reads=["top", tbk], writes=["top"])
                    tr.op("dve", lambda e, in0=in0, in1=in1: e.tensor_tensor(
                        out=cand[:].rearrange("p h (a b) -> p h a b", a=16), in0=in0, in1=in1, op=ALU.mult),
                        reads=["top"], writes=["cand"])
                    for h in range(8):
                        tr.op("dve", lambda e, h=h: e.max(out=cvv[:, h, 0:8], in_=cand[:, h, :]), reads=["cand"], writes=["cvv"])
                        tr.op("dve", lambda e, h=h: e.match_replace(out=cand2[:, h, :], in_to_replace=cvv[:, h, 0:8],
                                                                   in_values=cand[:, h, :], imm_value=-1.0),
                              reads=["cand", "cvv"], writes=["cand2"])
                        tr.op("dve", lambda e, h=h: e.max(out=cvv[:, h, 8:16], in_=cand2[:, h, :]), reads=["cand2"], writes=["cvv"])
                    tr.op("dve", lambda e, tb=tb: e.tensor_scalar(out=tb[:, 0:8], in0=cvv[:, :, 15], scalar1=1.0 - 1e-6, scalar2=None,
                                                                 op0=ALU.mult), reads=["cvv"], writes=[tbk])
                    def st_(tt=tt, et=et, etk=etk, ets=ets, tb=tb, tbk=tbk, tbs=tbs):
                        tr.dma("act", ets, sc_d[tt * 128:(tt + 1) * 128, :], et[:].rearrange("p a b -> p (a b)"), reads=[etk], writes=["sc_d"])
                        tr.dma("act", tbs, tb_d[tt * 128:(tt + 1) * 128, :], tb[:], reads=[tbk], writes=["tb_d"])
                    pending_stores.append(st_)
                while pending_stores:
                    pending_stores.pop(0)()
                tr.barrier()

        def phase_peer():
            with ExitStack() as ph:
                sb, ps = mk_alloc(ph)
                gB = sb("gB3", [128, D], F32)
                s_g = tr.slot("gB3")
                tr.dma("sp", s_g, gB[:], bcast_rows(I("final_norm_g"), D), writes=["gB3"])
                tr.barrier()
                xr = Ring(tr, sb, "xt2_ld", 2, [128, 16, 128], BF16)
                scr = Ring(tr, sb, "sc2_ld", 1, [128, 16, 128], F32)
                tbr = Ring(tr, sb, "tb2_ld", 2, [128, 16], F32)
                Wb = [sb("Wsum0", [128, 128, 128], BF16), sb("Wsum1", [128, 128, 128], BF16)]
                Sb = Ring(tr, sb, "S_b", 2, [128, 16, 128], F32, slots=False)
                Mb = Ring(tr, sb, "M_b", 2, [128, 16, 128], BF16, slots=False)
                ur = Ring(tr, sb, "uT_ld", 3, [128, 16, 256], BF16)
                vr = Ring(tr, sb, "vb_ld", 3, [128, 2, D], BF16)
                gar = Ring(tr, sb, "gA", 2, [128, 256], BF16, slots=False)
                ggr = Ring(tr, sb, "gG", 2, [128, 256], BF16, slots=False)
                gtr = Ring(tr, sb, "gT", 3, [128, 2, 128], BF16, slots=False)
                aps = Ring(tr, ps, "a_ps", 2, [128, 512], F32, slots=False)
                tps = Ring(tr, ps, "gt_ps", 2, [128, 8, 128], BF16, slots=False)
                yps = ps("y_ps", [128, 4, 512], F32)
                hr = Ring(tr, sb, "h1_ld", 1, [128, D], F32)
                junk = sb("junk3", [128, D], BF16)
                ssq = sb("ssq3", [128, 1], F32)
                xn2T_v = xn2T_d.rearrange("c p t -> p c t")
                vb_v = vb_d.rearrange("(i j) d -> j i d", j=128)
                NCH = 64
                tile_in = {}
                l_q = {}

                def load_tile(tt):
                    xt, xk, xs = xr.next()
                    tr.dma("sp", xs, xt[:], xn2T_v[:, :, tt * 128:(tt + 1) * 128], writes=[xk])
                    sc, sck, scs = scr.next()
                    tr.dma("sp", scs, sc[:].rearrange("p a b -> p (a b)"), sc_d[tt * 128:(tt + 1) * 128, :], writes=[sck])
                    tb, tbk, tbs = tbr.next()
                    tr.dma("sp", tbs, tb[:], tb_d[tt * 128:(tt + 1) * 128, :], writes=[tbk])
                    tile_in[tt] = (xt, xk, sc, sck, tb, tbk)

                def build_step(tt, step):
                    _, _, sc, sck, tb, tbk = tile_in[tt]
                    W = Wb[tt % 2]
                    eighth, h = step // 8, step % 8
                    i0 = eighth * 16
                    S, Sk, _ = Sb.next()
                    in0 = sc[:, 2 * h, i0:i0 + 16].unsqueeze(2).to_broadcast([128, 16, 128])
                    in1 = sc[:, 2 * h + 1, :].unsqueeze(1).to_broadcast([128, 16, 128])
                    if step % 4 == 3:
                        tr.op("dve", lambda e: e.tensor_tensor(out=S[:], in0=in0, in1=in1, op=ALU.mult), reads=[sck], writes=[Sk])
                    else:
                        def outer(e):
                            for ii in range(16):
                                ins = e.activation(out=S[:, ii, :], in_=sc[:, 2 * h + 1, :], func=AF.Copy,
                                                   scale=sc[:, 2 * h, i0 + ii:i0 + ii + 1])
                            return ins
                        tr.op("act", outer, reads=[sck], writes=[Sk])
                    wv = W[:, i0:i0 + 16, :]
                    wkey = "W%d_%d" % (tt % 2, eighth)
                    if h == 0:
                        tr.op("dve", lambda e: e.scalar_tensor_tensor(out=wv, in0=S[:], scalar=tb[:, h:h + 1], in1=S[:],
                                                                     op0=ALU.is_ge, op1=ALU.mult),
                              reads=[Sk, tbk], writes=[wkey])
                    else:
                        M, Mk, _ = Mb.next()
                        tr.op("dve", lambda e: e.scalar_tensor_tensor(out=M[:], in0=S[:], scalar=tb[:, h:h + 1], in1=S[:],
                                                                     op0=ALU.is_ge, op1=ALU.mult),
                              reads=[Sk, tbk], writes=[Mk])
                        tr.op("dve", lambda e: e.tensor_tensor(out=wv, in0=wv, in1=M[:], op=ALU.add), reads=[Mk, wkey], writes=[wkey])

                load_tile(0)
                for step in range(64):
                    build_step(0, step)
                for tt in range(16):
                    xt, xk, sc, sck, tb, tbk = tile_in[tt]
                    if tt + 1 < 16:
                        load_tile(tt + 1)
                    hb, hk, hs = hr.next()
                    tr.dma("sp", hs, hb[:], h1_d[tt * 128:(tt + 1) * 128, :], writes=[hk])
                    Wf = Wb[tt % 2][:].rearrange("p i j -> p (i j)")

                    def stageL(t2, ch):
                        ub, uk, us = ur.next()
                        tr.dma("sp", us, ub[:], uT_d[ch], writes=[uk])
                        vb_, vk, vs = vr.next()
                        tr.dma("sp", vs, vb_[:], vb_v[:, ch * 2:(ch + 1) * 2, :], writes=[vk])
                        l_q[(t2, ch)] = (ub, uk, vb_, vk)

                    def stageA(ch, xt=xt, xk=xk, Wf=Wf, tt=tt):
                        ub, uk, vb_, vk = l_q.pop((tt, ch))
                        ap_, apk, _ = aps.next()

                        def mm(e):
                            for c in range(16):
                                ins = e.matmul(ap_[:, 0:256], lhsT=xt[:, c, :], rhs=ub[:, c, :], start=(c == 0), stop=(c == 15))
                            return ins
                        tr.op("pe", mm, reads=[xk, uk], writes=[apk])
                        ga, gak, _ = gar.next()
                        tr.op("act", lambda e: e.activation(out=ga[:], in_=ap_[:, 0:256], func=AF.Gelu), reads=[apk], writes=[gak])
                        gg, ggk, _ = ggr.next()
                        wkey = "W%d_%d" % (tt % 2, ch // 8)
                        tr.op("pool", lambda e: e.tensor_tensor(out=gg[:], in0=ga[:], in1=Wf[:, ch * 256:(ch + 1) * 256], op=ALU.mult),
                              reads=[gak, wkey], writes=[ggk])
                        return gg, ggk, vb_, vk

                    def stageT(gg, ggk, vb_, vk):
                        tp_, tpk, _ = tps.next()

                        def tp(e):
                            for j in range(2):
                                ins = e.transpose(out=tp_[:, j, :], in_=gg[:, j * 128:(j + 1) * 128], identity=ident_b[:])
                            return ins
                        tr.op("pe", tp, reads=[ggk], writes=[tpk])
                        gt, gtk, _ = gtr.next()
                        tr.op("act", lambda e: e.copy(out=gt[:], in_=tp_[:, 0:2, :]), reads=[tpk], writes=[gtk])
                        return gt, gtk, vb_, vk

                    def stageY(ch, gt, gtk, vb_, vk):
                        def mm(e):
                            for ib in range(2):
                                for dc in range(4):
                                    ins = e.matmul(yps[:, dc, :], lhsT=gt[:, ib, :], rhs=vb_[:, ib, dc * 512:(dc + 1) * 512],
                                                   start=(ch == 0 and ib == 0), stop=(ch == NCH - 1 and ib == 1))
                            return ins
                        tr.op("pe", mm, reads=[gtk, vk], writes=["yps"])

                    a_q = {}
                    t_q = {}
                    if tt == 0:
                        stageL(0, 0)
                        stageL(0, 1)
                    a_q[0] = stageA(0)
                    for ch in range(NCH):
                        if ch + 1 < NCH:
                            a_q[ch + 1] = stageA(ch + 1)
                        t_q[ch] = stageT(*a_q.pop(ch))
                        if ch >= 1:
                            stageY(ch - 1, *t_q.pop(ch - 1))
                        if ch + 2 < NCH:
                            stageL(tt, ch + 2)
                        elif tt + 1 < 16:
                            stageL(tt + 1, ch + 2 - NCH)
                        if tt + 1 < 16:
                            build_step(tt + 1, ch)
                    stageY(NCH - 1, *t_q.pop(NCH - 1))
                    for dc in range(4):
                        tr.op("dve", lambda e, dc=dc, hb=hb: e.tensor_tensor(out=hb[:, dc * 512:(dc + 1) * 512], in0=yps[:, dc, :],
                                                                          in1=hb[:, dc * 512:(dc + 1) * 512], op=ALU.add),
                              reads=["yps", hk], writes=[hk])
                    tr.op("act", lambda e, hb=hb: e.activation(out=junk[:], in_=hb[:], func=AF.Square, accum_out=ssq[:]),
                          reads=[hk], writes=["junk3", "ssq3"])
                    tr.op("act", lambda e: e.activation(out=ssq[:], in_=ssq[:], func=AF.Sqrt, scale=1.0 / D, bias=EPS),
                          reads=["ssq3"], writes=["ssq3"])
                    tr.op("dve", lambda e: e.reciprocal(out=ssq[:], in_=ssq[:]), reads=["ssq3"], writes=["ssq3"])
                    tr.op("dve", lambda e, hb=hb: e.scalar_tensor_tensor(out=hb[:], in0=hb[:], scalar=ssq[:, 0:1], in1=gB[:],
                                                                       op0=ALU.mult, op1=ALU.mult),
                          reads=[hk, "ssq3", "gB3"], writes=[hk])
                    tr.dma("act", hs, out[tt * 128:(tt + 1) * 128, :], hb[:], reads=[hk], writes=["out"])
                tr.barrier()

        if upto >= 1:
            phase_proj(False)
            phase_proj(True)
        if upto >= 3:
            with ExitStack() as cstack:
                cst = conv_pre(cstack)
                phase_attn(do_vcast=(upto >= 5), hook=lambda: conv_hook(cst, 8))
                phase_conv_mix(cst)
                tr.barrier()
        elif upto >= 2:
            phase_attn(do_vcast=(upto >= 5))
        if upto >= 4:
            phase_wout()
        if upto >= 5:
            phase_route()
        if upto >= 6:
            tr.barrier(join_bg=True)
            phase_peer()

        tr.finish("sp")
    return nc


def _rel_bucket_np(rel):
    rel = np.asarray(rel).astype(np.int32)
    n = np.abs(rel)
    nf = np.maximum(n, 1).astype(np.float32)
    large = 8 + (np.log(nf / np.float32(8)) / np.float32(math.log(16)) * np.float32(8)).astype(np.int32)
    large = np.minimum(large, 15)
    return (rel > 0).astype(np.int32) * 16 + np.where(n < 8, n, large)


def make_in_maps(inputs, names=None):
    f = lambda a: np.ascontiguousarray(np.asarray(a, dtype=np.float32))
    x = f(inputs["x"])
    def chunked(w, nk):
        w = np.asarray(w, dtype=np.float32)
        nch = w.shape[1] // 256
        return np.ascontiguousarray(w.reshape(nk, 128, nch, 256).transpose(2, 1, 0, 3))

    def pk(v):
        v = np.asarray(v, dtype=np.float32)
        return np.ascontiguousarray(v.reshape(-1, 128).T)
    shared = {
        "w_in": chunked(inputs["w_in"][0], 16), "norm_mix_g": f(inputs["norm_mix_g"][0]), "b_gate": pk(inputs["b_gate"][0]),
        "lam4": f(np.stack([np.asarray(inputs[k][0]) for k in ("lam_q1", "lam_k1", "lam_q2", "lam_k2")])),
        "subln_g": f(inputs["subln_g"][0]), "w_att_out": chunked(inputs["w_att_out"][0], 8),
        "conv_w": np.ascontiguousarray(np.asarray(inputs["conv_w"][0], dtype=np.float32).reshape(31, 8, 128).transpose(2, 1, 0)),
        "cvec_in": np.ascontiguousarray(np.stack([pk(inputs[k][0]) for k in ("conv_b", "conv_ln_g", "conv_ln_b")], axis=1)),
        "w_conv_out": chunked(inputs["w_conv_out"][0], 8), "w_out": f(inputs["w_out"][0]),
        "rel_bias": f(inputs["rel_bias"]), "norm_ffn_g": f(inputs["norm_ffn_g"][0]),
        "peer_w_q": f(inputs["peer_w_q"][0]),
        "peer_sub_keys": f(np.asarray(inputs["peer_sub_keys"][0]).reshape(16, 128, 128)),
        "peer_u": f(inputs["peer_u"][0]), "peer_v": f(inputs["peer_v"][0]),
        "final_norm_g": f(inputs["final_norm_g"]),
    }
    rp = np.arange(512)
    bk = _rel_bucket_np(256 - rp)
    oh = np.zeros((32, 512), np.float32)
    oh[bk, rp] = 1.0
    shared["oh_r"] = oh
    shared["iota128"] = np.tile(np.arange(128, dtype=np.float32)[None, :], (128, 1))
    maps = []
    for core in range(8):
        b, s = core // 2, core % 2
        m = dict(shared)
        m["x_own"] = np.ascontiguousarray(x[b, s * T:(s + 1) * T])
        m["x_pre"] = np.ascontiguousarray(x[b, 0:T])
        fl = np.zeros((128, 2), np.float32)
        fl[:, 0] = 1.0 if s == 1 else 0.0
        fl[:, 1] = 0.0 if s == 1 else NEG
        m["flags"] = fl
        if names is not None:
            m = {k: v for k, v in m.items() if k in names}
        maps.append(m)
    return maps


def kernel(**inputs):
    nc = build()
    in_maps = make_in_maps(inputs)
    used = set(nc._used_inputs.keys())
    in_maps = [{k: v for k, v in m.items() if k in used} for m in in_maps]
    res = run_bass_kernel_spmd(nc, in_maps, core_ids=list(range(8)))
    outp = np.zeros((4, 4096, D), np.float32)
    for core in range(8):
        b, s = core // 2, core % 2
        outp[b, s * T:(s + 1) * T] = res.results[core]["out"]
    return outp
```

```python
import math
import numpy as np
import ml_dtypes
from contextlib import ExitStack
import concourse.bass as bass
import concourse.mybir as mybir
from concourse.bass_utils import run_bass_kernel_spmd

F32 = mybir.dt.float32
BF16 = mybir.dt.bfloat16
U32 = mybir.dt.uint32
AF = mybir.ActivationFunctionType
ALU = mybir.AluOpType
AX = mybir.AxisListType

D = 2048
T = 2048
NEG = -1e30
EPS = 1e-6
LAM_INIT = 0.8 - 0.6 * math.exp(0.0)


class Res:
    def __init__(self, sem, step):
        self.sem = sem
        self.step = step
        self.count = 0


class Eng:
    def __init__(self, name, h, res):
        self.name = name
        self.h = h
        self.res = res
        self.seen = {}


class Tracker:
    def __init__(self, nc, es):
        self.nc = nc
        self.es = es
        self.last_write = {}
        self.readers = {}
        self.engs = {}
        for name, h in (("pe", nc.tensor), ("act", nc.scalar), ("dve", nc.vector),
                        ("pool", nc.gpsimd), ("sp", nc.sync)):
            sem = es.enter_context(nc.semaphore("sem_" + name))
            self.engs[name] = Eng(name, h, Res(sem, 1))
        self.all_res = [e.res for e in self.engs.values()]
        self.nslots = 0
        self.bg = set()

    def slot(self, name):
        sem = self.es.enter_context(self.nc.semaphore("dq_" + name))
        r = Res(sem, 16)
        self.all_res.append(r)
        self.nslots += 1
        return r

    def _need(self, eng, stamp):
        res, val = stamp
        if eng.seen.get(res, 0) >= val:
            return
        eng.h.wait_ge(res.sem, val)
        eng.seen[res] = val

    def _deps(self, eng, own, reads, writes):
        for k in reads:
            st = self.last_write.get(k)
            if st is not None:
                self._need(eng, st)
        for k in writes:
            st = self.last_write.get(k)
            if st is not None and st[0] is not own:
                self._need(eng, st)
            for r, v in self.readers.get(k, {}).items():
                if r is not own:
                    self._need(eng, (r, v))

    def _commit(self, stamp, reads, writes):
        for k in reads:
            d = self.readers.setdefault(k, {})
            d[stamp[0]] = max(d.get(stamp[0], 0), stamp[1])
        for k in writes:
            self.last_write[k] = stamp
            self.readers[k] = {}

    def op(self, ename, fn, reads=(), writes=()):
        eng = self.engs[ename]
        self._deps(eng, eng.res, reads, writes)
        ins = fn(eng.h)
        eng.res.count += 1
        ins.then_inc(eng.res.sem, 1)
        self._commit((eng.res, eng.res.count), reads, writes)
        return ins

    def dma(self, qname, slot, out, in_, reads=(), writes=(), **kw):
        eng = self.engs[qname]
        self._deps(eng, None, reads, writes)
        ins = eng.h.dma_start(out=out, in_=in_, **kw)
        slot.count += 16
        ins.then_inc(slot.sem, 16)
        self._commit((slot, slot.count), reads, writes)
        return ins

    def barrier(self, join_bg=False):
        if join_bg:
            self.bg = set()
        for eng in self.engs.values():
            for r in self.all_res:
                if r.count > 0 and r not in self.bg:
                    self._need(eng, (r, r.count))
        keep_w = {k: v for k, v in self.last_write.items() if v[0] in self.bg}
        self.last_write = keep_w
        self.readers = {}

    def finish(self, ename="sp"):
        eng = self.engs[ename]
        for r in self.all_res:
            if r.count > 0:
                self._need(eng, (r, r.count))


class Ring:
    def __init__(self, tr, alloc, name, n, shape, dt, slots=True):
        self.bufs = [alloc(f"{name}{i}", shape, dt) for i in range(n)]
        self.keys = [f"{name}{i}" for i in range(n)]
        self.slots = [tr.slot(f"{name}{i}") for i in range(n)] if slots else None
        self.i = -1
        self.n = n

    def next(self):
        self.i = (self.i + 1) % self.n
        return self.cur()

    def cur(self):
        return self.bufs[self.i], self.keys[self.i], (self.slots[self.i] if self.slots else None)


def bcast_rows(ap1d, n):
    return bass.AP(ap1d.tensor, ap1d.offset, [[0, 128], [1, n]])


def build(upto=99, debug=()):
    nc = bass.Bass("TRN2", target_bir_lowering=False)

    def din(name, shape, dt=F32):
        return nc.dram_tensor(name, list(shape), dt, kind="ExternalInput").ap()

    def scratch(name, shape, dt):
        kind = "ExternalOutput" if name in debug else "Internal"
        return nc.dram_tensor(name, list(shape), dt, kind=kind).ap()

    SHAPES = {
        "x_own": [T, D], "x_pre": [T, D], "w_in": [36, 128, 16, 256], "norm_mix_g": [D], "b_gate": [128, 32],
        "lam4": [4, 64], "subln_g": [128], "w_att_out": [8, 128, 8, 256], "conv_w": [128, 8, 31], "cvec_in": [128, 3, 8],
        "w_conv_out": [8, 128, 8, 256], "w_out": [D, D],
        "rel_bias": [32, 8], "norm_ffn_g": [D], "peer_w_q": [D, D], "peer_sub_keys": [16, 128, 128],
        "peer_u": [16384, D], "peer_v": [16384, D], "final_norm_g": [D], "oh_r": [32, 512],
        "iota128": [128, 128], "flags": [128, 2],
    }
    _decl = {}

    def I(name):
        if name not in _decl:
            _decl[name] = din(name, SHAPES[name])
        return _decl[name]
    nc._used_inputs = _decl
    out = nc.dram_tensor("out", [T, D], F32, kind="ExternalOutput").ap()

    qT_d = scratch("qT_d", [1024, T], BF16)
    kT_d = scratch("kT_d", [1024, 2 * T], BF16)
    v_d = scratch("v_d", [2 * T, 1024], BF16)
    hT_d = scratch("hT_d", [1024, 32 + T], F32)
    gates_d = scratch("gates_d", [4096, T], BF16)
    gr_d = scratch("gr_d", [8, 512], F32)
    oT_d = scratch("oT_d", [1024, T], BF16)
    mixT_d = scratch("mixT_d", [D, T], BF16)
    h1_d = scratch("h1_d", [T, D], F32)
    xn2T_d = scratch("xn2T_d", [16, 128, T], BF16)
    uT_d = scratch("uT_d", [64, 128, 16, 256], BF16)
    vb_d = scratch("vb_d", [16384, D], BF16)

    dbgP = scratch("dbgP", [128, 2, 512], BF16)
    dbgO = scratch("dbgO", [128, 3, 512], F32)
    dbgE = scratch("dbgE", [128, 260], F32)
    with ExitStack() as es:
        es.enter_context(nc.Block())
        tr = Tracker(nc, es)

        def mk_alloc(stack):
            def sb(name, shape, dt):
                return stack.enter_context(nc.sbuf_tensor(name, list(shape), dt))

            def ps(name, shape, dt):
                return stack.enter_context(nc.psum_tensor(name, list(shape), dt))
            return sb, ps

        gsb, gps = mk_alloc(es)
        s_c = tr.slot("const")
        ident_f = gsb("ident_f", [128, 128], F32)
        ident_b = gsb("ident_b", [128, 128], BF16)
        anti_b = gsb("anti_b", [128, 128], BF16)
        iota_f = gsb("iota_f", [128, 128], F32)
        flags = gsb("flags_s", [128, 2], F32)
        ones_b = gsb("ones_b", [128, 128], BF16)
        ones_f = gsb("ones_f", [128, 128], F32)
        tr.op("pool", lambda e: e.memset(ident_f[:], 0.0), writes=["ident_f"])
        tr.op("pool", lambda e: e.affine_select(out=ident_f[:], in_=ident_f[:], pattern=[[-1, 128]],
                                               compare_op=ALU.not_equal, fill=1.0, base=0, channel_multiplier=1),
              reads=["ident_f"], writes=["ident_f"])
        tr.op("dve", lambda e: e.tensor_copy(out=ident_b[:], in_=ident_f[:]), reads=["ident_f"], writes=["ident_b"])
        tr.op("pool", lambda e: e.memset(anti_b[:], 0.0), writes=["anti_b"])
        tr.op("pool", lambda e: e.affine_select(out=anti_b[:], in_=anti_b[:], pattern=[[1, 128]],
                                               compare_op=ALU.not_equal, fill=1.0, base=-127, channel_multiplier=1),
              reads=["anti_b"], writes=["anti_b"])
        tr.op("pool", lambda e: e.memset(ones_b[:], 1.0), writes=["ones_b"])
        tr.op("pool", lambda e: e.memset(ones_f[:], 1.0), writes=["ones_f"])
        tr.dma("sp", s_c, iota_f[:], I("iota128"), writes=["iota_f"])
        tr.dma("sp", s_c, flags[:], I("flags"), writes=["flags"])
        CONST = ["ident_f", "ident_b", "anti_b", "iota_f", "flags", "ones_b", "ones_f"]

        def const_barrier():
            tr.barrier()

        vcast_state = {}

        def vcast_piece(i):
            if "slot" not in vcast_state:
                vcast_state["slot"] = tr.slot("vcast")
                tr.bg.add(vcast_state["slot"])
            for q4 in range(4):
                r0 = i * 1024 + q4 * 256
                tr.dma("pool", vcast_state["slot"], vb_d[r0:r0 + 256, :], I("peer_v")[r0:r0 + 256, :], writes=["vb_d"])

        def make_table_emitter(sb, ps):
            uring = Ring(tr, sb, "u_ld", 2, [128, D], BF16, slots=False)
            ufr = Ring(tr, sb, "u_f32", 2, [128, D], F32)
            stg = Ring(tr, sb, "u_st", 2, [128, 16, 512], BF16)
            ptr = Ring(tr, ps, "u_pt", 2, [128, 8, 128], BF16, slots=False)
            st = {"n": 0, "sg": None}

            def block(eb):
                uf, ufk, ufs = ufr.next()
                tr.dma("sp", ufs, uf[:], I("peer_u")[eb * 128:(eb + 1) * 128, :], writes=[ufk])
                ub, uk, _ = uring.next()
                tr.op("act", lambda e, ub=ub, uf=uf: e.copy(out=ub[:], in_=uf[:]), reads=[ufk], writes=[uk])
                if eb % 4 == 0:
                    st["sg"] = stg.next()
                sg, sk, ss = st["sg"]
                for g in range(4):
                    pt, pk, _ = ptr.next()

                    def tp(e, g=g, ub=ub, pt=pt):
                        for j in range(4):
                            c = g * 4 + j
                            ins = e.transpose(out=pt[:, j, :], in_=ub[:, c * 128:(c + 1) * 128], identity=ident_b[:])
                        return ins
                    tr.op("pe", tp, reads=[uk, "ident_b"], writes=[pk])
                    st["n"] += 1
                    o = sg[:, g * 4:(g + 1) * 4, (eb % 4) * 128:(eb % 4 + 1) * 128]
                    tr.op("act", lambda e, o=o, pt=pt: e.copy(out=o, in_=pt[:, 0:4, :]), reads=[pk], writes=[sk])
                if eb % 4 == 3:
                    for hh in range(2):
                        tr.dma("act", ss, uT_d[(eb // 4) * 2 + hh], sg[:, :, hh * 256:(hh + 1) * 256],
                               reads=[sk], writes=["uT_d"])
            return block

        def phase_proj(own):
            xsrc = I("x_own") if own else I("x_pre")
            tag = "o" if own else "p"
            with ExitStack() as ph:
                sb, ps = mk_alloc(ph)
                xnT = sb("xnT" + tag, [128, 16, T], BF16)
                gB = sb("gB" + tag, [128, D], F32)
                s_g = tr.slot("gB" + tag)
                tr.dma("sp", s_g, gB[:], bcast_rows(I("norm_mix_g"), D), writes=["gB"])
                with ExitStack() as p0:
                    sb0, ps0 = mk_alloc(p0)
                    xr = Ring(tr, sb0, "x_ld" + tag, 3, [128, D], F32)
                    xnr = Ring(tr, sb0, "xn" + tag, 2, [128, D], BF16, slots=False)
                    junk = sb0("junk" + tag, [128, D], BF16)
                    ssr = Ring(tr, sb0, "ssq" + tag, 2, [128, 1], F32, slots=False)
                    rsr = Ring(tr, sb0, "rstd" + tag, 2, [128, 1], F32, slots=False)
                    ptr = Ring(tr, ps0, "x_pt" + tag, 2, [128, 8, 128], BF16, slots=False)
                    n = 0
                    for tt in range(16):
                        xb, xk, xs = xr.next()
                        tr.dma("sp", xs, xb[:], xsrc[tt * 128:(tt + 1) * 128, :], writes=[xk])
                        sq, sqk, _ = ssr.next()
                        rs, rsk, _ = rsr.next()
                        xn, xnk, _ = xnr.next()
                        tr.op("act", lambda e, xb=xb, sq=sq: e.activation(out=junk[:], in_=xb[:], func=AF.Square, accum_out=sq[:]),
                              reads=[xk], writes=["junk", sqk])
                        tr.op("act", lambda e, rs=rs, sq=sq: e.activation(out=rs[:], in_=sq[:], func=AF.Sqrt, scale=1.0 / D, bias=EPS),
                              reads=[sqk], writes=[rsk])
                        tr.op("dve", lambda e, rs=rs: e.reciprocal(out=rs[:], in_=rs[:]), reads=[rsk], writes=[rsk])
                        tr.op("dve", lambda e, xn=xn, xb=xb, rs=rs: e.scalar_tensor_tensor(
                            out=xn[:], in0=xb[:], scalar=rs[:, 0:1], in1=gB[:], op0=ALU.mult, op1=ALU.mult),
                            reads=[xk, rsk, "gB"], writes=[xnk])
                        for g in range(4):
                            pt, pk, _ = ptr.next()

                            def tp(e, g=g, xn=xn, pt=pt):
                                for j in range(4):
                                    c = g * 4 + j
                                    ins = e.transpose(out=pt[:, j, :], in_=xn[:, c * 128:(c + 1) * 128], identity=ident_b[:])
                                return ins
                            tr.op("pe", tp, reads=[xnk, "ident_b"], writes=[pk])
                            o = xnT[:, g * 4:(g + 1) * 4, tt * 128:(tt + 1) * 128]
                            if n % 2 == 0:
                                tr.op("dve", lambda e, o=o, pt=pt: e.tensor_copy(out=o, in_=pt[:, 0:4, :]), reads=[pk], writes=["xnT"])
                            else:
                                tr.op("act", lambda e, o=o, pt=pt: e.copy(out=o, in_=pt[:, 0:4, :]), reads=[pk], writes=["xnT"])
                            n += 1
                    tr.barrier()
                wr = Ring(tr, sb, "w_ld" + tag, 3, [128, 16, 256], BF16)
                pps = Ring(tr, ps, "pj_ps" + tag, 4, [128, 512], F32, slots=False)
                st16 = Ring(tr, sb, "pj_st" + tag, 3, [128, T], BF16)
                st32 = Ring(tr, sb, "pj_sf" + tag, 2, [128, T], F32)
                sig = Ring(tr, sb, "pj_sig" + tag, 2, [128, 512], F32, slots=False)
                vst = Ring(tr, sb, "pj_vs" + tag, 2, [128, 256], BF16)
                bg = sb("bg" + tag, [128, 32], F32)
                s_bg = tr.slot("bg" + tag)
                tr.dma("sp", s_bg, bg[:], I("b_gate"), writes=["bg"])
                w_view = I("w_in")
                cnt = [0]

                def load_w(col0):
                    wb, wk, ws = wr.next()
                    tr.dma("pool", ws, wb[:], w_view[col0 // 256], writes=[wk])
                    return wb, wk

                def fm_group(wb, wk, cb, tg, ntok=512, tok0=None):
                    pp, ppk, _ = pps.next()
                    t0 = tg * 512 if tok0 is None else tok0

                    def mm(e, pp=pp):
                        for c in range(16):
                            ins = e.matmul(pp[:, 0:ntok], lhsT=wb[:, c, cb * 128:(cb + 1) * 128],
                                           rhs=xnT[:, c, t0:t0 + ntok], start=(c == 0), stop=(c == 15))
                        return ins
                    tr.op("pe", mm, reads=[wk, "xnT"], writes=[ppk])
                    return pp, ppk

                def evac(pp, ppk, o, ok, scale=None):
                    cnt[0] += 1
                    if cnt[0] % 2 == 0:
                        if scale is None:
                            tr.op("dve", lambda e: e.tensor_copy(out=o, in_=pp[:, 0:o.shape[1]]), reads=[ppk], writes=[ok])
                        else:
                            tr.op("dve", lambda e: e.tensor_scalar(out=o, in0=pp[:, 0:o.shape[1]], scalar1=scale, scalar2=None,
                                                                  op0=ALU.mult), reads=[ppk], writes=[ok])
                    else:
                        if scale is None:
                            tr.op("act", lambda e: e.copy(out=o, in_=pp[:, 0:o.shape[1]]), reads=[ppk], writes=[ok])
                        else:
                            tr.op("act", lambda e: e.mul(out=o, in_=pp[:, 0:o.shape[1]], mul=scale), reads=[ppk], writes=[ok])

                plan = []
                if own:
                    plan += [("q", c0) for c0 in range(0, 1024, 256)]
                plan += [("k", 1024 + c0) for c0 in range(0, 1024, 256)]
                for kind, col0 in plan:
                    wb, wk = load_w(col0)
                    for cb in range(2):
                        stb, stk, sts = st16.next()
                        for tg in range(4):
                            pp, ppk = fm_group(wb, wk, cb, tg)
                            evac(pp, ppk, stb[:, tg * 512:(tg + 1) * 512], stk, scale=(0.125 if kind == "q" else None))
                        if kind == "q":
                            r0 = col0 + cb * 128
                            tr.dma("sp", sts, qT_d[r0:r0 + 128, :], stb[:], reads=[stk], writes=["qT_d"])
                        else:
                            r0 = col0 - 1024 + cb * 128
                            tcol = T if own else 0
                            tr.dma("sp", sts, kT_d[r0:r0 + 128, tcol:tcol + T], stb[:], reads=[stk], writes=["kT_d"])
                for c0 in range(0, 1024, 256):
                    wb, wk = load_w(2048 + c0)
                    for tt in range(16):
                        pp, ppk, _ = pps.next()

                        def mm(e, pp=pp, wb=wb, tt=tt):
                            for c in range(16):
                                ins = e.matmul(pp[:, 0:256], lhsT=xnT[:, c, tt * 128:(tt + 1) * 128], rhs=wb[:, c, :],
                                               start=(c == 0), stop=(c == 15))
                            return ins
                        tr.op("pe", mm, reads=[wk, "xnT"], writes=[ppk])
                        vb, vk, vs = vst.next()
                        evac(pp, ppk, vb[:], vk)
                        trow = (T if own else 0) + tt * 128
                        tr.dma("sp", vs, v_d[trow:trow + 128, c0:c0 + 256], vb[:], reads=[vk], writes=["v_d"])
                for c0 in range(0, 1024, 256):
                    wa, wak = load_w(3072 + c0)
                    wbb, wbk = load_w(4096 + c0)
                    for cb in range(2):
                        r0 = c0 + cb * 128
                        if own:
                            hb, hk, hs = st32.next()
                            for tg in range(4):
                                pa, pak = fm_group(wa, wak, cb, tg)
                                pb, pbk = fm_group(wbb, wbk, cb, tg)
                                sg, sgk, _ = sig.next()
                                tr.op("act", lambda e, sg=sg, pb=pb: e.activation(out=sg[:], in_=pb[:], func=AF.Sigmoid),
                                      reads=[pbk], writes=[sgk])
                                tr.op("dve", lambda e, hb=hb, pa=pa, sg=sg, tg=tg: e.tensor_tensor(
                                    out=hb[:, tg * 512:(tg + 1) * 512], in0=pa[:], in1=sg[:], op=ALU.mult),
                                    reads=[pak, sgk], writes=[hk])
                            tr.dma("sp", hs, hT_d[r0:r0 + 128, 32:32 + T], hb[:], reads=[hk], writes=["hT_d"])
                        else:
                            hb, hk, hs = st32.next()
                            pa, pak = fm_group(wa, wak, cb, 0, ntok=128, tok0=T - 128)
                            pb, pbk = fm_group(wbb, wbk, cb, 0, ntok=128, tok0=T - 128)
                            sg, sgk, _ = sig.next()
                            tr.op("act", lambda e, sg=sg, pb=pb: e.activation(out=sg[:, 0:128], in_=pb[:, 0:128], func=AF.Sigmoid),
                                  reads=[pbk], writes=[sgk])
                            tr.op("dve", lambda e, hb=hb, pa=pa, sg=sg: e.scalar_tensor_tensor(
                                out=hb[:, 0:128], in0=pa[:, 0:128], scalar=flags[:, 0:1], in1=sg[:, 0:128],
                                op0=ALU.mult, op1=ALU.mult), reads=[pak, sgk, "flags"], writes=[hk])
                            tr.dma("sp", hs, hT_d[r0:r0 + 128, 0:32], hb[:, 96:128], reads=[hk], writes=["hT_d"])
                if own:
                    for c0 in range(0, 4096, 256):
                        wb, wk = load_w(5120 + c0)
                        for cb in range(2):
                            blk = (c0 + cb * 128) // 128
                            stb, stk, sts = st16.next()
                            for tg in range(4):
                                pp, ppk = fm_group(wb, wk, cb, tg)
                                tr.op("act", lambda e, stb=stb, pp=pp, tg=tg, blk=blk: e.activation(
                                    out=stb[:, tg * 512:(tg + 1) * 512], in_=pp[:], func=AF.Sigmoid, bias=bg[:, blk:blk + 1]),
                                    reads=[ppk, "bg"], writes=[stk])
                            tr.dma("sp", sts, gates_d[blk * 128:(blk + 1) * 128, :], stb[:], reads=[stk], writes=["gates_d"])
                tr.barrier()


        def phase_attn(do_vcast=False, hook=None):
            with ExitStack() as ph:
                sb, ps = mk_alloc(ph)
                s_b = tr.slot("attn_setup")
                s_b2 = tr.slot("attn_setup2")
                lam_t = sb("lam_t", [128, 256], F32)
                lam4 = I("lam4")
                tr.dma("sp", s_b, lam_t[:], bass.AP(lam4.tensor, lam4.offset, [[0, 128], [1, 256]]), writes=["lam_t"])
                lp = sb("lam_p", [128, 128], F32)
                ls = sb("lam_s", [128, 2], F32)
                nlam = sb("nlam", [128, 1], F32)
                dummies = {en: sb("lam_dummy_" + en, [128, 512], F32) for en in ("dve", "act", "pool")}
                nl1 = sb("nl1", [128, 1], F32)
                ls2 = sb("lam_s2", [128, 2], F32)

                def settle():
                    tr.barrier()
                    for en in ("dve", "act", "pool"):
                        tr.op(en, lambda e, en=en: e.memset(dummies[en][:], 0.0) if en != "act" else e.memzero(dummies[en][:]), writes=["dummy_" + en])
                    tr.barrier()
                settle()
                tr.op("dve", lambda e: e.tensor_tensor(out=lp[:, 0:64], in0=lam_t[:, 0:64], in1=lam_t[:, 64:128], op=ALU.mult),
                      reads=["lam_t"], writes=["lp"])
                tr.op("dve", lambda e: e.tensor_tensor(out=lp[:, 64:128], in0=lam_t[:, 128:192], in1=lam_t[:, 192:256], op=ALU.mult),
                      reads=["lam_t"], writes=["lp"])
                settle()
                tr.op("dve", lambda e: e.reduce_sum(out=ls[:, 0:1], in_=lp[:, 0:64], axis=AX.X), reads=["lp"], writes=["ls"])
                tr.op("dve", lambda e: e.reduce_sum(out=ls[:, 1:2], in_=lp[:, 64:128], axis=AX.X), reads=["lp"], writes=["ls"])
                settle()
                tr.op("act", lambda e: e.activation(out=ls2[:], in_=ls[:], func=AF.Exp), reads=["ls"], writes=["ls2"])
                settle()
                tr.op("dve", lambda e: e.tensor_tensor(out=nl1[:], in0=ls2[:, 1:2], in1=ls2[:, 0:1], op=ALU.subtract),
                      reads=["ls2"], writes=["nl1"])
                settle()
                tr.op("dve", lambda e: e.tensor_scalar(out=nlam[:], in0=nl1[:], scalar1=-LAM_INIT, scalar2=None, op0=ALU.add),
                      reads=["nl1"], writes=["nlam"])
                settle()
                rb = sb("rb", [32, 8], F32)
                ohs = sb("ohs", [32, 512], F32)
                grs = sb("grs", [8, 512], F32)
                tr.dma("sp", tr.slot("attn_rb"), rb[:], I("rel_bias"), writes=["rb"])
                tr.dma("sp", tr.slot("attn_oh"), ohs[:], I("oh_r"), writes=["ohs"])
                with ExitStack() as p1:
                    _, ps1 = mk_alloc(p1)
                    gps_ = ps1("gr_ps", [8, 512], F32)
                    tr.op("pe", lambda e: e.matmul(gps_[:], lhsT=rb[:], rhs=ohs[:], start=True, stop=True),
                          reads=["rb", "ohs"], writes=["gr_ps"])
                    tr.op("dve", lambda e: e.tensor_copy(out=grs[:], in_=gps_[:]), reads=["gr_ps"], writes=["grs"])
                    tr.dma("sp", s_b2, gr_d, grs[:], reads=["grs"], writes=["gr_d"])
                    tr.barrier()
                hank = sb("hank", [128, 8, 2, 128], F32)
                for h in range(8):
                    tr.dma("sp", s_b, hank[:, h, 0, :], bass.AP(gr_d.tensor, h * 512 + 129, [[1, 128], [1, 128]]),
                           reads=["gr_d"], writes=["hank"])
                    tr.dma("sp", s_b, hank[:, h, 1, :], bass.AP(gr_d.tensor, h * 512 + 257, [[1, 128], [1, 128]]),
                           reads=["gr_d"], writes=["hank"])
                cB = sb("cB", [128, 8], F32)
                cBm = sb("cBm", [128, 8], F32)
                relb = I("rel_bias")
                tr.dma("sp", s_b, cB[:], bass.AP(relb.tensor, relb.offset + 15 * 8, [[0, 128], [1, 8]]), writes=["cB"])
                sgB = sb("sgB", [128, 128], F32)
                tr.dma("sp", s_b, sgB[:], bcast_rows(I("subln_g"), 128), writes=["sgB"])
                tr.barrier()
                tr.op("dve", lambda e: e.tensor_scalar(out=sgB[:], in0=sgB[:], scalar1=1.0 - LAM_INIT, scalar2=None, op0=ALU.mult),
                      reads=["sgB"], writes=["sgB"])
                tr.op("dve", lambda e: e.tensor_scalar(out=cBm[:], in0=cB[:], scalar1=flags[:, 1:2], scalar2=None, op0=ALU.add),
                      reads=["cB", "flags"], writes=["cBm"])
                tiles = sb("btiles", [128, 8, 5, 128], BF16)
                tr.op("dve", lambda e: e.tensor_copy(out=tiles[:, :, 0, :], in_=hank[:, :, 0, :]), reads=["hank"], writes=["tiles"])
                tr.op("dve", lambda e: e.tensor_copy(out=tiles[:, :, 1, :], in_=hank[:, :, 1, :]), reads=["hank"], writes=["tiles"])
                tr.op("dve", lambda e: e.tensor_copy(out=tiles[:, :, 2, :], in_=cB[:].unsqueeze(2).to_broadcast([128, 8, 128])),
                      reads=["cB"], writes=["tiles"])
                tr.op("dve", lambda e: e.tensor_scalar(out=tiles[:, :, 3, :], in0=hank[:, :, 1, :], scalar1=flags[:, 1:2], scalar2=None,
                                                      op0=ALU.add), reads=["hank", "flags"], writes=["tiles"])
                tr.op("dve", lambda e: e.tensor_copy(out=tiles[:, :, 4, :], in_=cBm[:].unsqueeze(2).to_broadcast([128, 8, 128])),
                      reads=["cBm"], writes=["tiles"])
                tr.barrier()
                tr.op("dve", lambda e: e.memset(tiles[0:64, :, 0, 0:64], NEG), writes=["tiles"])
                tr.barrier()

                kr = Ring(tr, sb, "kT_h", 2, [128, 2 * T], BF16)
                qr = Ring(tr, sb, "qT_h", 2, [128, T], BF16)
                vr = Ring(tr, sb, "v_h", 2, [128, 32, 129], BF16)
                for vb_ in vr.bufs:
                    tr.op("pool", lambda e, vb_=vb_: e.memset(vb_[:, :, 128:129], 1.0), writes=["vones"])
                tr.barrier()
                sring = Ring(tr, ps, "s_ps", 2, [128, 2, 512], F32, slots=False)
                oA = [ps("oA0", [128, 512], F32), ps("oA1", [128, 512], F32)]
                oC = ps("oC", [128, 512], F32)
                tps = ps("o_tp", [128, 128], BF16)
                ptr_ = Ring(tr, sb, "pT", 3, [128, 2, 512], BF16, slots=False)
                ostg = Ring(tr, sb, "oT_st", 2, [128, T], BF16)
                rz = sb("rz", [128, 4], F32)
                rzA = sb("rzA", [128, 1], F32)
                t0 = sb("o_t0", [128, 128], F32)
                ob = sb("o_ob", [128, 128], F32)
                ojunk = sb("o_junk", [128, 128], F32)
                onb = sb("o_onb", [128, 128], BF16)

                if "dbgO" in debug:
                    dbgO_s = sb("dbgO_s", [128, 3, 512], F32)

                def oreg(m, j):
                    if j < 3:
                        return oA[m][:, j * 160:j * 160 + 129]
                    return oC[:, m * 256:m * 256 + 129]

                for h in range(8):
                    if do_vcast:
                        vcast_piece(2 * h)
                        vcast_piece(2 * h + 1)
                    kb_, kk, ks = kr.next()
                    qb_, qk_, qs = qr.next()
                    vb_, vk, vs = vr.next()
                    tr.dma("sp", ks, kb_[:], kT_d[h * 128:(h + 1) * 128, :], writes=[kk])
                    tr.dma("sp", qs, qb_[:], qT_d[h * 128:(h + 1) * 128, :], writes=[qk_])
                    tr.dma("sp", vs, vb_[:, :, 0:128], v_d[:, h * 128:(h + 1) * 128].rearrange("(kb p) e -> p kb e", p=128),
                           writes=[vk])
                    osb, osk, oss = ostg.next()
                    for g in range(4):
                        Q0 = 16 + 4 * g
                        nkb = Q0 + 4

                        def qk(kb, g=g, Q0=Q0, h=h, kb_=kb_, qb_=qb_, kk=kk, qk_=qk_):
                            jlo = max(0, kb - Q0)
                            mixed = kb >= Q0 - 1
                            S, Sk, _ = sring.next()

                            def mm(e):
                                for m in range(2):
                                    ins = e.matmul(S[:, m, jlo * 128:512], lhsT=kb_[m * 64:(m + 1) * 64, kb * 128:(kb + 1) * 128],
                                                   rhs=qb_[m * 64:(m + 1) * 64, g * 512 + jlo * 128:(g + 1) * 512],
                                                   start=True, stop=not mixed)
                                if mixed:
                                    for j in range(jlo, 4):
                                        if kb == Q0 + j:
                                            kind = 0
                                        elif kb == Q0 + j - 1:
                                            kind = 3 if kb < 16 else 1
                                        else:
                                            kind = 4 if kb < 16 else 2
                                        for m in range(2):
                                            ins = e.matmul(S[:, m, j * 128:(j + 1) * 128], lhsT=anti_b[:], rhs=tiles[:, h, kind, :],
                                                           start=False, stop=(j == 3))
                                return ins
                            tr.op("pe", mm, reads=[kk, qk_], writes=[Sk])
                            P, Pk, _ = ptr_.next()
                            if mixed:
                                tr.op("act", lambda e: e.activation(out=P[:, :, jlo * 128:512], in_=S[:, :, jlo * 128:512], func=AF.Exp),
                                      reads=[Sk], writes=[Pk])
                            else:
                                bias = (cB if kb >= 16 else cBm)[:, h:h + 1]
                                tr.op("act", lambda e: e.activation(out=P[:], in_=S[:], func=AF.Exp, bias=bias),
                                      reads=[Sk], writes=[Pk])
                            if "dbgP" in debug and h == 0 and g == 0 and kb == 3:
                                tr.dma("sp", s_b2, dbgP, P[:], reads=[Pk], writes=["dbgP"])
                            return P, Pk, jlo

                        def pv(kb, P, Pk, jlo, Q0=Q0, vb_=vb_, vk=vk):
                            def mm(e):
                                for j in range(jlo, 4):
                                    for m in range(2):
                                        ins = e.matmul(oreg(m, j), lhsT=P[:, m, j * 128:(j + 1) * 128], rhs=vb_[:, kb, :],
                                                       start=(kb == 0 and (j == 0 or (j == 3 and m == 0))),
                                                       stop=((j == 2 and kb == Q0 + 2) or (j == 3 and m == 1 and kb == Q0 + 3)))
                                return ins
                            tr.op("pe", mm, reads=[Pk, vk], writes=["oacc"])

                        pend = None
                        for kb in range(nkb):
                            cur = qk(kb)
                            if pend is not None:
                                pv(*pend)
                            pend = (kb,) + cur
                        pv(*pend)
                        if hook is not None:
                            hook()
                        if "dbgO" in debug and h == 0 and g == 0:
                            for ii, src in enumerate((oA[0], oA[1], oC)):
                                tr.op("dve", lambda e, ii=ii, src=src: e.tensor_copy(out=dbgO_s[:, ii, :], in_=src[:]), reads=["oacc"], writes=["dbgO_s"])
                            tr.dma("sp", s_b2, dbgO, dbgO_s[:], reads=["dbgO_s"], writes=["dbgO"])
                        for j in range(4):
                            r0, r1 = oreg(0, j), oreg(1, j)
                            tr.op("dve", lambda e: e.reciprocal(out=rz[:, 0:1], in_=r0[:, 128:129]), reads=["oacc"], writes=["rz"])
                            tr.op("dve", lambda e: e.reciprocal(out=rz[:, 1:2], in_=r1[:, 128:129]), reads=["oacc"], writes=["rz"])
                            tr.op("dve", lambda e: e.tensor_scalar(out=t0[:], in0=r0[:, 0:128], scalar1=rz[:, 0:1], scalar2=None, op0=ALU.mult),
                                  reads=["oacc", "rz"], writes=["t0"])
                            tr.op("dve", lambda e: e.tensor_tensor(out=rz[:, 2:3], in0=rz[:, 1:2], in1=nlam[:], op=ALU.mult),
                                  reads=["rz"], writes=["rz2"])
                            tr.op("dve", lambda e: e.scalar_tensor_tensor(out=ob[:], in0=r1[:, 0:128], scalar=rz[:, 2:3], in1=t0[:],
                                                                         op0=ALU.mult, op1=ALU.add),
                                  reads=["oacc", "rz2", "t0"], writes=["ob"])
                            tr.op("act", lambda e: e.activation(out=ojunk[:], in_=ob[:], func=AF.Square, accum_out=rzA[:]),
                                  reads=["ob"], writes=["ojunk", "rz3"])
                            tr.op("act", lambda e: e.activation(out=rzA[:], in_=rzA[:], func=AF.Sqrt, scale=1.0 / 128, bias=EPS),
                                  reads=["rz3"], writes=["rz3"])
                            tr.op("dve", lambda e: e.reciprocal(out=rzA[:], in_=rzA[:]), reads=["rz3"], writes=["rz3"])
                            tr.op("dve", lambda e: e.scalar_tensor_tensor(out=onb[:], in0=ob[:], scalar=rzA[:, 0:1], in1=sgB[:],
                                                                         op0=ALU.mult, op1=ALU.mult),
                                  reads=["ob", "rz3"], writes=["onb"])
                            tr.op("pe", lambda e: e.transpose(out=tps[:], in_=onb[:], identity=ident_b[:]), reads=["onb"], writes=["tps"])
                            if "dbgE" in debug and h == 0 and g == 0 and j == 0:
                                tr.dma("sp", s_b2, dbgE[:, 0:128], ob[:], reads=["ob"], writes=["dbgE"])
                                tr.dma("sp", s_b2, dbgE[:, 128:132], rz[:], reads=["rz", "rz2"], writes=["dbgE"])
                                tr.dma("sp", s_b2, dbgE[:, 132:260], t0[:], reads=["t0"], writes=["dbgE"])
                                tr.barrier()
                            c0 = (4 * g + j) * 128
                            tr.op("dve", lambda e, c0=c0: e.tensor_copy(out=osb[:, c0:c0 + 128], in_=tps[:]), reads=["tps"], writes=[osk])
                    tr.dma("act", oss, oT_d[h * 128:(h + 1) * 128, :], osb[:], reads=[osk], writes=["oT_d"])
                tr.barrier()


        def conv_pre(stack):
            sbc, _ = mk_alloc(stack)
            s_c1 = tr.slot("conv_setup")
            cw = sbc("cw", [128, 8, 31], F32)
            cvec = sbc("cvec", [128, 3, 8], F32)
            tr.dma("sp", s_c1, cw[:], I("conv_w"), writes=["cw"])
            tr.dma("sp", tr.slot("conv_setup2"), cvec[:], I("cvec_in"), writes=["cvec"])
            cv = sbc("convo", [128, 8, T], F32)
            hr = Ring(tr, sbc, "h_ld", 2, [128, 32 + T], F32)
            ops = []
            cur = {}

            def mk(k, j):
                def f():
                    if j == 0:
                        hb, hk, hs = hr.next()
                        tr.dma("sp", hs, hb[:], hT_d[k * 128:(k + 1) * 128, :], writes=[hk])
                        cur["h"] = (hb, hk)
                    hb, hk = cur["h"]
                    src = hb[:, 2 + j:2 + j + T]
                    if j == 0:
                        tr.op("dve", lambda e: e.tensor_scalar(
                            out=cv[:, k, :], in0=src, scalar1=cw[:, k, j:j + 1], scalar2=cvec[:, 0, k:k + 1],
                            op0=ALU.mult, op1=ALU.add), reads=[hk, "cw", "cvec"], writes=["cv%d" % k])
                    else:
                        tr.op("dve", lambda e: e.scalar_tensor_tensor(
                            out=cv[:, k, :], in0=src, scalar=cw[:, k, j:j + 1], in1=cv[:, k, :],
                            op0=ALU.mult, op1=ALU.add), reads=[hk, "cw", "cv%d" % k], writes=["cv%d" % k])
                return f
            for k in range(8):
                for j in range(31):
                    ops.append(mk(k, j))
            return {"cw": cw, "cvec": cvec, "cv": cv, "ops": ops}

        def conv_hook(cst, n=8):
            for _ in range(n):
                if cst["ops"]:
                    cst["ops"].pop(0)()

        def phase_conv_mix(cst):
            with ExitStack() as ph:
                sb, ps = mk_alloc(ph)
                sT = sb("sT", [128, 8, T], BF16)
                with ExitStack() as pc:
                    sbc, psc = mk_alloc(pc)
                    cw, cvec, cv = cst["cw"], cst["cvec"], cst["cv"]
                    while cst["ops"]:
                        cst["ops"].pop(0)()
                    sqr = Ring(tr, sbc, "c_sq", 2, [128, T], F32, slots=False)
                    mean_ps = psc("mean_ps", [128, 4, 512], F32)
                    ex2_ps = psc("ex2_ps", [128, 4, 512], F32)
                    for k in range(8):
                        sq, sqk, _ = sqr.next()
                        tr.op("act", lambda e, sq=sq, k=k: e.activation(out=sq[:], in_=cv[:, k, :], func=AF.Square),
                              reads=["cv%d" % k], writes=[sqk])

                        def st(e, k=k, sq=sq):
                            for tg in range(4):
                                e.matmul(mean_ps[:, tg, :], lhsT=ones_f[:], rhs=cv[:, k, tg * 512:(tg + 1) * 512],
                                         start=(k == 0), stop=(k == 7))
                                ins = e.matmul(ex2_ps[:, tg, :], lhsT=ones_f[:], rhs=sq[:, tg * 512:(tg + 1) * 512],
                                               start=(k == 0), stop=(k == 7))
                            return ins
                        tr.op("pe", st, reads=["cv%d" % k, sqk, "ones_f"], writes=["stats_ps"])
                    mean = sbc("c_mean", [128, T], F32)
                    rstd = sbc("c_rstd", [128, T], F32)
                    tr.op("act", lambda e: e.mul(out=mean[:], in_=mean_ps[:].rearrange("p a b -> p (a b)"), mul=1.0 / 1024),
                          reads=["stats_ps"], writes=["mean"])
                    tr.op("dve", lambda e: e.tensor_tensor(out=rstd[:], in0=mean[:], in1=mean[:], op=ALU.mult),
                          reads=["mean"], writes=["rstd"])
                    tr.op("dve", lambda e: e.scalar_tensor_tensor(out=rstd[:], in0=ex2_ps[:].rearrange("p a b -> p (a b)"),
                                                                 scalar=1.0 / 1024, in1=rstd[:], op0=ALU.mult, op1=ALU.subtract),
                          reads=["stats_ps", "rstd"], writes=["rstd"])
                    tr.op("act", lambda e: e.activation(out=rstd[:], in_=rstd[:], func=AF.Sqrt, bias=EPS),
                          reads=["rstd"], writes=["rstd"])
                    tr.op("dve", lambda e: e.reciprocal(out=rstd[:], in_=rstd[:]), reads=["rstd"], writes=["rstd"])
                    for k in range(8):
                        tr.op("dve", lambda e, k=k: e.tensor_tensor(out=cv[:, k, :], in0=cv[:, k, :], in1=mean[:], op=ALU.subtract),
                              reads=["cv%d" % k, "mean"], writes=["cv%d" % k])
                        tr.op("pool", lambda e, k=k: e.tensor_tensor(out=cv[:, k, :], in0=cv[:, k, :], in1=rstd[:], op=ALU.mult),
                              reads=["cv%d" % k, "rstd"], writes=["cv%d" % k])
                        tr.op("act", lambda e, k=k: e.activation(out=sT[:, k, :], in_=cv[:, k, :], func=AF.Silu,
                                                                scale=cvec[:, 1, k:k + 1], bias=cvec[:, 2, k:k + 1]),
                              reads=["cv%d" % k, "cvec"], writes=["sT"])
                    tr.barrier()
                oT = sb("oT_s", [128, 8, T], BF16)
                s_o = tr.slot("oT_ld")
                tr.dma("sp", s_o, oT[:], oT_d.rearrange("(k p) t -> p k t", p=128), writes=["oT"])
                war = Ring(tr, sb, "wao", 2, [128, 8, 256], BF16)
                wcr = Ring(tr, sb, "wco", 2, [128, 8, 256], BF16)
                gar = Ring(tr, sb, "g_a", 2, [128, T], BF16)
                gcr = Ring(tr, sb, "g_c", 2, [128, T], BF16)
                mst = Ring(tr, sb, "mix_st", 2, [128, T], BF16)
                t1r = Ring(tr, sb, "mix_t1", 2, [128, 512], F32, slots=False)
                t2r = Ring(tr, sb, "mix_t2", 2, [128, 512], F32, slots=False)
                pa = Ring(tr, ps, "ya_ps", 2, [128, 512], F32, slots=False)
                pc_ = Ring(tr, ps, "yc_ps", 2, [128, 512], F32, slots=False)
                wao_v = I("w_att_out")
                wco_v = I("w_conv_out")
                for f2 in range(8):
                    wa, wak, was = war.next()
                    wc, wck, wcs = wcr.next()
                    tr.dma("pool", was, wa[:], wao_v[f2], writes=[wak])
                    tr.dma("pool", wcs, wc[:], wco_v[f2], writes=[wck])
                    for fb in range(2):
                        f = f2 * 2 + fb
                        ga, gak, gas = gar.next()
                        gc, gck, gcs = gcr.next()
                        tr.dma("sp", gas, ga[:], gates_d[f * 128:(f + 1) * 128, :], writes=[gak])
                        tr.dma("sp", gcs, gc[:], gates_d[2048 + f * 128:2048 + (f + 1) * 128, :], writes=[gck])
                        mb, mk, ms = mst.next()
                        for tg in range(4):
                            pA, pAk, _ = pa.next()
                            pC, pCk, _ = pc_.next()

                            def mm(e, pA=pA, pC=pC, wa=wa, wc=wc, fb=fb, tg=tg):
                                for k in range(8):
                                    e.matmul(pA[:], lhsT=wa[:, k, fb * 128:(fb + 1) * 128], rhs=oT[:, k, tg * 512:(tg + 1) * 512],
                                             start=(k == 0), stop=(k == 7))
                                for k in range(8):
                                    ins = e.matmul(pC[:], lhsT=wc[:, k, fb * 128:(fb + 1) * 128], rhs=sT[:, k, tg * 512:(tg + 1) * 512],
                                                   start=(k == 0), stop=(k == 7))
                                return ins
                            tr.op("pe", mm, reads=[wak, wck, "oT", "sT"], writes=[pAk, pCk])
                            t1, t1k, _ = t1r.next()
                            t2, t2k, _ = t2r.next()
                            tsl = slice(tg * 512, (tg + 1) * 512)
                            tr.op("dve", lambda e, t1=t1, pA=pA, ga=ga, tsl=tsl: e.tensor_tensor(out=t1[:], in0=pA[:], in1=ga[:, tsl], op=ALU.mult),
                                  reads=[pAk, gak], writes=[t1k])
                            tr.op("dve", lambda e, t2=t2, pC=pC, gc=gc, tsl=tsl: e.tensor_tensor(out=t2[:], in0=pC[:], in1=gc[:, tsl], op=ALU.mult),
                                  reads=[pCk, gck], writes=[t2k])
                            tr.op("pool", lambda e, mb=mb, t1=t1, t2=t2, tsl=tsl: e.tensor_tensor(out=mb[:, tsl], in0=t1[:], in1=t2[:], op=ALU.add),
                                  reads=[t1k, t2k], writes=[mk])
                        tr.dma("act", ms, mixT_d[f * 128:(f + 1) * 128, :], mb[:], reads=[mk], writes=["mixT_d"])
                tr.barrier()

        def phase_wout():
            with ExitStack() as ph:
                sb, ps = mk_alloc(ph)
                wo = sb("wo_s", [128, 16, D], BF16)
                s_w = tr.slot("wo_ld")
                wo_v = I("w_out").rearrange("(c p) n -> p c n", p=128)
                for c4 in range(4):
                    tr.dma("pool", s_w, wo[:, c4 * 4:(c4 + 1) * 4, :], wo_v[:, c4 * 4:(c4 + 1) * 4, :], writes=["wo"])
                gB = sb("gB2", [128, D], F32)
                tr.dma("sp", tr.slot("gB2_ld"), gB[:], bcast_rows(I("norm_ffn_g"), D), writes=["gB2"])
                tr.barrier()
                mr = Ring(tr, sb, "mixT_ld", 2, [128, 16, 512], BF16)
                xr = Ring(tr, sb, "x2_ld", 2, [128, D], F32)
                hr = Ring(tr, sb, "h1_st", 2, [128, D], F32)
                xnr = Ring(tr, sb, "xn2", 2, [128, D], BF16, slots=False)
                junk = sb("junk2", [128, D], BF16)
                ssr = Ring(tr, sb, "ssq2", 2, [128, 1], F32, slots=False)
                xts = Ring(tr, sb, "xn2T_st", 2, [128, 16, 512], BF16)
                pps = Ring(tr, ps, "wo_ps", 4, [128, 512], F32, slots=False)
                ptr = Ring(tr, ps, "x2_pt", 2, [128, 8, 128], BF16, slots=False)
                mix_v = mixT_d.rearrange("(c p) t -> p c t", p=128)
                n = 0
                for tg in range(4):
                    mb, mk, ms = mr.next()
                    tr.dma("sp", ms, mb[:], mix_v[:, :, tg * 512:(tg + 1) * 512], writes=[mk])
                    xt, xtk, xtsl = xts.next()
                    for t4 in range(4):
                        tt = tg * 4 + t4
                        xb, xk, xs = xr.next()
                        tr.dma("sp", xs, xb[:], I("x_own")[tt * 128:(tt + 1) * 128, :], writes=[xk])
                        hb, hk, hs = hr.next()
                        for dc in range(4):
                            pp, ppk, _ = pps.next()

                            def mm(e, pp=pp, mb=mb, t4=t4, dc=dc):
                                for c in range(16):
                                    ins = e.matmul(pp[:], lhsT=mb[:, c, t4 * 128:(t4 + 1) * 128], rhs=wo[:, c, dc * 512:(dc + 1) * 512],
                                                   start=(c == 0), stop=(c == 15))
                                return ins
                            tr.op("pe", mm, reads=[mk, "wo"], writes=[ppk])
                            tr.op("dve", lambda e, hb=hb, pp=pp, xb=xb, dc=dc: e.tensor_tensor(
                                out=hb[:, dc * 512:(dc + 1) * 512], in0=pp[:], in1=xb[:, dc * 512:(dc + 1) * 512], op=ALU.add),
                                reads=[ppk, xk], writes=[hk])
                        tr.dma("act", hs, h1_d[tt * 128:(tt + 1) * 128, :], hb[:], reads=[hk], writes=["h1_d"])
                        sq, sqk, _ = ssr.next()
                        xn, xnk, _ = xnr.next()
                        tr.op("act", lambda e, hb=hb, sq=sq: e.activation(out=junk[:], in_=hb[:], func=AF.Square, accum_out=sq[:]),
                              reads=[hk], writes=["junk2", sqk])
                        tr.op("act", lambda e, sq=sq: e.activation(out=sq[:], in_=sq[:], func=AF.Sqrt, scale=1.0 / D, bias=EPS),
                              reads=[sqk], writes=[sqk])
                        tr.op("dve", lambda e, sq=sq: e.reciprocal(out=sq[:], in_=sq[:]), reads=[sqk], writes=[sqk])
                        tr.op("dve", lambda e, xn=xn, hb=hb, sq=sq: e.scalar_tensor_tensor(
                            out=xn[:], in0=hb[:], scalar=sq[:, 0:1], in1=gB[:], op0=ALU.mult, op1=ALU.mult),
                            reads=[hk, sqk, "gB2"], writes=[xnk])
                        for g in range(4):
                            pt, pk, _ = ptr.next()

                            def tp(e, g=g, xn=xn, pt=pt):
                                for j in range(4):
                                    c = g * 4 + j
                                    ins = e.transpose(out=pt[:, j, :], in_=xn[:, c * 128:(c + 1) * 128], identity=ident_b[:])
                                return ins
                            tr.op("pe", tp, reads=[xnk], writes=[pk])
                            o = xt[:, g * 4:(g + 1) * 4, t4 * 128:(t4 + 1) * 128]
                            if n % 2 == 0:
                                tr.op("dve", lambda e, o=o, pt=pt: e.tensor_copy(out=o, in_=pt[:, 0:4, :]), reads=[pk], writes=[xtk])
                            else:
                                tr.op("act", lambda e, o=o, pt=pt: e.copy(out=o, in_=pt[:, 0:4, :]), reads=[pk], writes=[xtk])
                            n += 1
                    tr.dma("act", xtsl, xn2T_d[:, :, tg * 512:(tg + 1) * 512].rearrange("c p t -> p c t"), xt[:],
                           reads=[xtk], writes=["xn2T_d"])
                tr.barrier()


        sc_d = scratch("sc_d", [T, 2048], F32)
        tb_d = scratch("tb_d", [T, 16], F32)

        def phase_route():
            with ExitStack() as ph:
                sb, ps = mk_alloc(ph)
                wq = sb("wq_s", [128, 16, D], BF16)
                s_w = tr.slot("wq_ld")
                wq_v = I("peer_w_q").rearrange("(c p) n -> p c n", p=128)
                for c4 in range(4):
                    tr.dma("pool", s_w, wq[:, c4 * 4:(c4 + 1) * 4, :], wq_v[:, c4 * 4:(c4 + 1) * 4, :], writes=["wq"])
                skn = sb("skn", [128, 16, 128], BF16)
                skT = sb("skT", [128, 16, 128], BF16)
                tr.dma("pool", s_w, skn[:], I("peer_sub_keys").rearrange("b n c -> n b c"), writes=["skn"])
                tr.barrier()
                ptr = Ring(tr, ps, "sk_pt", 2, [128, 8, 128], BF16, slots=False)
                for g in range(4):
                    pt, pk, _ = ptr.next()

                    def tp(e, g=g, pt=pt):
                        for j in range(4):
                            ins = e.transpose(out=pt[:, j, :], in_=skn[:, g * 4 + j, :], identity=ident_b[:])
                        return ins
                    tr.op("pe", tp, reads=["skn"], writes=[pk])
                    tr.op("dve", lambda e, g=g, pt=pt: e.tensor_copy(out=skT[:, g * 4:(g + 1) * 4, :], in_=pt[:, 0:4, :]), reads=[pk], writes=["skT"])
                tr.barrier()
                xr = Ring(tr, sb, "xt_ld", 2, [128, 16, 128], BF16)
                qps = Ring(tr, ps, "q_ps", 2, [128, 4, 128], F32, slots=False)
                sps = Ring(tr, ps, "sc_ps", 2, [128, 4, 128], F32, slots=False)
                qTp = sb("qTp", [128, 16, 128], BF16)
                scr = Ring(tr, sb, "sc_s", 2, [128, 16, 128], F32)
                sc2 = sb("sc2", [128, 16, 128], F32)
                mx = sb("mx", [128, 16], F32)
                etr = Ring(tr, sb, "et_s", 2, [128, 16, 128], F32)
                top = sb("top", [128, 16, 16], F32)
                cand = sb("cand", [128, 8, 256], F32)
                cand2 = sb("cand2", [128, 8, 256], F32)
                cvv = sb("cvv", [128, 8, 16], F32)
                ee = sb("ee", [128, 8, 16], F32)
                zz = sb("zz", [128, 8], F32)
                tbr = Ring(tr, sb, "tb_s", 2, [128, 16], F32)
                xn2T_v = xn2T_d.rearrange("c p t -> p c t")
                tblock = make_table_emitter(sb, ps)
                pending_stores = []
                for tt in range(16):
                    for eb in range(tt * 8, tt * 8 + 8):
                        tblock(eb)
                    while pending_stores:
                        pending_stores.pop(0)()
                    xt, xk, xs = xr.next()
                    tr.dma("sp", xs, xt[:], xn2T_v[:, :, tt * 128:(tt + 1) * 128], writes=[xk])
                    for g in range(4):
                        qp, qpk, _ = qps.next()

                        def mm(e, g=g, qp=qp, xt=xt):
                            for j in range(4):
                                blk = g * 4 + j
                                for c in range(16):
                                    ins = e.matmul(qp[:, j, :], lhsT=wq[:, c, blk * 128:(blk + 1) * 128], rhs=xt[:, c, :],
                                                   start=(c == 0), stop=(c == 15))
                            return ins
                        tr.op("pe", mm, reads=["wq", xk], writes=[qpk])
                        tr.op("act", lambda e, g=g, qp=qp: e.copy(out=qTp[:, g * 4:(g + 1) * 4, :], in_=qp[:]), reads=[qpk], writes=["qTp%d" % g])
                    sc, sck, scs = scr.next()
                    for g in range(4):
                        sp_, spk, _ = sps.next()

                        def mm2(e, g=g, sp_=sp_):
                            for j in range(4):
                                blk = g * 4 + j
                                ins = e.matmul(sp_[:, j, :], lhsT=qTp[:, blk, :], rhs=skT[:, blk, :], start=True, stop=True)
                            return ins
                        tr.op("pe", mm2, reads=["qTp%d" % g, "skT"], writes=[spk])
                        tr.op("act", lambda e, g=g, sp_=sp_, sc=sc: e.copy(out=sc[:, g * 4:(g + 1) * 4, :], in_=sp_[:]), reads=[spk], writes=[sck])
                    tr.op("dve", lambda e, sc=sc: e.tensor_reduce(out=mx[:], in_=sc[:], axis=AX.X, op=ALU.max), reads=[sck], writes=["mx"])
                    tr.op("dve", lambda e, sc=sc: e.tensor_tensor(out=sc2[:], in0=sc[:], in1=mx[:].unsqueeze(2).to_broadcast([128, 16, 128]),
                                                                 op=ALU.subtract), reads=[sck, "mx"], writes=["sc2"])
                    et, etk, ets = etr.next()
                    tr.op("act", lambda e, et=et: e.activation(out=et[:], in_=sc2[:], func=AF.Exp), reads=["sc2"], writes=[etk])
                    for blk in range(16):
                        tr.op("dve", lambda e, blk=blk, et=et: e.max(out=top[:, blk, 0:8], in_=et[:, blk, :]), reads=[etk], writes=["top"])
                        tr.op("dve", lambda e, blk=blk, et=et: e.match_replace(out=sc2[:, blk, :], in_to_replace=top[:, blk, 0:8],
                                                                             in_values=et[:, blk, :], imm_value=-1.0),
                              reads=[etk, "top"], writes=["sc2"])
                        tr.op("dve", lambda e, blk=blk: e.max(out=top[:, blk, 8:16], in_=sc2[:, blk, :]), reads=["sc2"], writes=["top"])
                    topv = top[:].rearrange("p (h two) k -> p h two k", two=2)
                    in0 = topv[:, :, 0, :].unsqueeze(3).to_broadcast([128, 8, 16, 16])
                    in1 = topv[:, :, 1, :].unsqueeze(2).to_broadcast([128, 8, 16, 16])
                    tr.op("dve", lambda e, in0=in0, in1=in1: e.tensor_tensor(
                        out=cand[:].rearrange("p h (a b) -> p h a b", a=16), in0=in0, in1=in1, op=ALU.mult),
                        reads=["top"], writes=["cand"])
                    for h in range(8):
                        tr.op("dve", lambda e, h=h: e.max(out=cvv[:, h, 0:8], in_=cand[:, h, :]), reads=["cand"], writes=["cvv"])
                        tr.op("dve", lambda e, h=h: e.match_replace(out=cand2[:, h, :], in_to_replace=cvv[:, h, 0:8],
                                                                   in_values=cand[:, h, :], imm_value=-1.0),
                              reads=["cand", "cvv"], writes=["cand2"])
                        tr.op("dve", lambda e, h=h: e.max(out=cvv[:, h, 8:16], in_=cand2[:, h, :]), reads=["cand2"], writes=["cvv"])
                    tb, tbk, tbs = tbr.next()
                    tr.op("dve", lambda e: e.reduce_sum(out=zz[:], in_=cvv[:], axis=AX.X), reads=["cvv"], writes=["zz"])
                    tr.op("dve", lambda e, tb=tb: e.reciprocal(out=tb[:, 8:16], in_=zz[:]), reads=["zz"], writes=[tbk])
                    etv = et[:].rearrange("p (h two) n -> p h two n", two=2)
                    tr.op("dve", lambda e, tb=tb, etv=etv: e.tensor_tensor(out=etv[:, :, 0, :], in0=etv[:, :, 0, :],
                                                                       in1=tb[:, 8:16].unsqueeze(2).to_broadcast([128, 8, 128]), op=ALU.mult),
                          reads=[etk, tbk], writes=[etk])
                    tr.op("dve", lambda e, tb=tb: e.tensor_tensor(out=topv[:, :, 0, :], in0=topv[:, :, 0, :],
                                                                 in1=tb[:, 8:16].unsqueeze(2).to_broadcast([128, 8, 16]), op=ALU.mult),
                          reads=["top", tbk], writes=["top"])
                    tr.op("dve", lambda e, in0=in0, in1=in1: e.tensor_tensor(
                        out=cand[:].rearrange("p h (a b) -> p h a b", a=16), in0=in0, in1=in1, op=ALU.mult),
                        reads=["top"], writes=["cand"])
                    for h in range(8):
                        tr.op("dve", lambda e, h=h: e.max(out=cvv[:, h, 0:8], in_=cand[:, h, :]), reads=["cand"], writes=["cvv"])
                        tr.op("dve", lambda e, h=h: e.match_replace(out=cand2[:, h, :], in_to_replace=cvv[:, h, 0:8],
                                                                   in_values=cand[:, h, :], imm_value=-1.0),
                              reads=["cand", "cvv"], writes=["cand2"])
                        tr.op("dve", lambda e, h=h: e.max(out=cvv[:, h, 8:16], in_=cand2[:, h, :]), reads=["cand2"], writes=["cvv"])
                    tr.op("dve", lambda e, tb=tb: e.tensor_scalar(out=tb[:, 0:8], in0=cvv[:, :, 15], scalar1=1.0 - 1e-6, scalar2=None,
                                                                 op0=ALU.mult), reads=["cvv"], writes=[tbk])
                    def st_(tt=tt, et=et, etk=etk, ets=ets, tb=tb, tbk=tbk, tbs=tbs):
                        tr.dma("act", ets, sc_d[tt * 128:(tt + 1) * 128, :], et[:].rearrange("p a b -> p (a b)"), reads=[etk], writes=["sc_d"])
                        tr.dma("act", tbs, tb_d[tt * 128:(tt + 1) * 128, :], tb[:], reads=[tbk], writes=["tb_d"])
                    pending_stores.append(st_)
                while pending_stores:
                    pending_stores.pop(0)()
                tr.barrier()

        def phase_peer():
            with ExitStack() as ph:
                sb, ps = mk_alloc(ph)
                gB = sb("gB3", [128, D], F32)
                s_g = tr.slot("gB3")
                tr.dma("sp", s_g, gB[:], bcast_rows(I("final_norm_g"), D), writes=["gB3"])
                tr.barrier()
                xr = Ring(tr, sb, "xt2_ld", 2, [128, 16, 128], BF16)
                scr = Ring(tr, sb, "sc2_ld", 1, [128, 16, 128], F32)
                tbr = Ring(tr, sb, "tb2_ld", 2, [128, 16], F32)
                Wb = [sb("Wsum0", [128, 128, 128], BF16), sb("Wsum1", [128, 128, 128], BF16)]
                Sb = Ring(tr, sb, "S_b", 2, [128, 16, 128], F32, slots=False)
                Mb = Ring(tr, sb, "M_b", 2, [128, 16, 128], BF16, slots=False)
                ur = Ring(tr, sb, "uT_ld", 3, [128, 16, 256], BF16)
                vr = Ring(tr, sb, "vb_ld", 3, [128, 2, D], BF16)
                gar = Ring(tr, sb, "gA", 2, [128, 256], BF16, slots=False)
                ggr = Ring(tr, sb, "gG", 2, [128, 256], BF16, slots=False)
                gtr = Ring(tr, sb, "gT", 3, [128, 2, 128], BF16, slots=False)
                aps = Ring(tr, ps, "a_ps", 2, [128, 512], F32, slots=False)
                tps = Ring(tr, ps, "gt_ps", 2, [128, 8, 128], BF16, slots=False)
                yps = ps("y_ps", [128, 4, 512], F32)
                hr = Ring(tr, sb, "h1_ld", 1, [128, D], F32)
                junk = sb("junk3", [128, D], BF16)
                ssq = sb("ssq3", [128, 1], F32)
                xn2T_v = xn2T_d.rearrange("c p t -> p c t")
                vb_v = vb_d.rearrange("(i j) d -> j i d", j=128)
                NCH = 64
                tile_in = {}
                l_q = {}

                def load_tile(tt):
                    xt, xk, xs = xr.next()
                    tr.dma("sp", xs, xt[:], xn2T_v[:, :, tt * 128:(tt + 1) * 128], writes=[xk])
                    sc, sck, scs = scr.next()
                    tr.dma("sp", scs, sc[:].rearrange("p a b -> p (a b)"), sc_d[tt * 128:(tt + 1) * 128, :], writes=[sck])
                    tb, tbk, tbs = tbr.next()
                    tr.dma("sp", tbs, tb[:], tb_d[tt * 128:(tt + 1) * 128, :], writes=[tbk])
                    tile_in[tt] = (xt, xk, sc, sck, tb, tbk)

                def build_step(tt, step):
                    _, _, sc, sck, tb, tbk = tile_in[tt]
                    W = Wb[tt % 2]
                    eighth, h = step // 8, step % 8
                    i0 = eighth * 16
                    S, Sk, _ = Sb.next()
                    in0 = sc[:, 2 * h, i0:i0 + 16].unsqueeze(2).to_broadcast([128, 16, 128])
                    in1 = sc[:, 2 * h + 1, :].unsqueeze(1).to_broadcast([128, 16, 128])
                    if step % 4 == 3:
                        tr.op("dve", lambda e: e.tensor_tensor(out=S[:], in0=in0, in1=in1, op=ALU.mult), reads=[sck], writes=[Sk])
                    else:
                        def outer(e):
                            for ii in range(16):
                                ins = e.activation(out=S[:, ii, :], in_=sc[:, 2 * h + 1, :], func=AF.Copy,
                                                   scale=sc[:, 2 * h, i0 + ii:i0 + ii + 1])
                            return ins
                        tr.op("act", outer, reads=[sck], writes=[Sk])
                    wv = W[:, i0:i0 + 16, :]
                    wkey = "W%d_%d" % (tt % 2, eighth)
                    if h == 0:
                        tr.op("dve", lambda e: e.scalar_tensor_tensor(out=wv, in0=S[:], scalar=tb[:, h:h + 1], in1=S[:],
                                                                     op0=ALU.is_ge, op1=ALU.mult),
                              reads=[Sk, tbk], writes=[wkey])
                    else:
                        M, Mk, _ = Mb.next()
                        tr.op("dve", lambda e: e.scalar_tensor_tensor(out=M[:], in0=S[:], scalar=tb[:, h:h + 1], in1=S[:],
                                                                     op0=ALU.is_ge, op1=ALU.mult),
                              reads=[Sk, tbk], writes=[Mk])
                        tr.op("dve", lambda e: e.tensor_tensor(out=wv, in0=wv, in1=M[:], op=ALU.add), reads=[Mk, wkey], writes=[wkey])

                load_tile(0)
                for step in range(64):
                    build_step(0, step)
                for tt in range(16):
                    xt, xk, sc, sck, tb, tbk = tile_in[tt]
                    if tt + 1 < 16:
                        load_tile(tt + 1)
                    hb, hk, hs = hr.next()
                    tr.dma("sp", hs, hb[:], h1_d[tt * 128:(tt + 1) * 128, :], writes=[hk])
                    Wf = Wb[tt % 2][:].rearrange("p i j -> p (i j)")

                    def stageL(t2, ch):
                        ub, uk, us = ur.next()
                        tr.dma("sp", us, ub[:], uT_d[ch], writes=[uk])
                        vb_, vk, vs = vr.next()
                        tr.dma("sp", vs, vb_[:], vb_v[:, ch * 2:(ch + 1) * 2, :], writes=[vk])
                        l_q[(t2, ch)] = (ub, uk, vb_, vk)

                    def stageA(ch, xt=xt, xk=xk, Wf=Wf, tt=tt):
                        ub, uk, vb_, vk = l_q.pop((tt, ch))
                        ap_, apk, _ = aps.next()

                        def mm(e):
                            for c in range(16):
                                ins = e.matmul(ap_[:, 0:256], lhsT=xt[:, c, :], rhs=ub[:, c, :], start=(c == 0), stop=(c == 15))
                            return ins
                        tr.op("pe", mm, reads=[xk, uk], writes=[apk])
                        ga, gak, _ = gar.next()
                        tr.op("act", lambda e: e.activation(out=ga[:], in_=ap_[:, 0:256], func=AF.Gelu), reads=[apk], writes=[gak])
                        gg, ggk, _ = ggr.next()
                        wkey = "W%d_%d" % (tt % 2, ch // 8)
                        tr.op("pool", lambda e: e.tensor_tensor(out=gg[:], in0=ga[:], in1=Wf[:, ch * 256:(ch + 1) * 256], op=ALU.mult),
                              reads=[gak, wkey], writes=[ggk])
                        return gg, ggk, vb_, vk

                    def stageT(gg, ggk, vb_, vk):
                        tp_, tpk, _ = tps.next()

                        def tp(e):
                            for j in range(2):
                                ins = e.transpose(out=tp_[:, j, :], in_=gg[:, j * 128:(j + 1) * 128], identity=ident_b[:])
                            return ins
                        tr.op("pe", tp, reads=[ggk], writes=[tpk])
                        gt, gtk, _ = gtr.next()
                        tr.op("act", lambda e: e.copy(out=gt[:], in_=tp_[:, 0:2, :]), reads=[tpk], writes=[gtk])
                        return gt, gtk, vb_, vk

                    def stageY(ch, gt, gtk, vb_, vk):
                        def mm(e):
                            for ib in range(2):
                                for dc in range(4):
                                    ins = e.matmul(yps[:, dc, :], lhsT=gt[:, ib, :], rhs=vb_[:, ib, dc * 512:(dc + 1) * 512],
                                                   start=(ch == 0 and ib == 0), stop=(ch == NCH - 1 and ib == 1))
                            return ins
                        tr.op("pe", mm, reads=[gtk, vk], writes=["yps"])

                    a_q = {}
                    t_q = {}
                    if tt == 0:
                        stageL(0, 0)
                        stageL(0, 1)
                    a_q[0] = stageA(0)
                    for ch in range(NCH):
                        if ch + 1 < NCH:
                            a_q[ch + 1] = stageA(ch + 1)
                        t_q[ch] = stageT(*a_q.pop(ch))
                        if ch >= 1:
                            stageY(ch - 1, *t_q.pop(ch - 1))
                        if ch + 2 < NCH:
                            stageL(tt, ch + 2)
                        elif tt + 1 < 16:
                            stageL(tt + 1, ch + 2 - NCH)
                        if tt + 1 < 16:
                            build_step(tt + 1, ch)
                    stageY(NCH - 1, *t_q.pop(NCH - 1))
                    for dc in range(4):
                        tr.op("dve", lambda e, dc=dc, hb=hb: e.tensor_tensor(out=hb[:, dc * 512:(dc + 1) * 512], in0=yps[:, dc, :],
                                                                          in1=hb[:, dc * 512:(dc + 1) * 512], op=ALU.add),
                              reads=["yps", hk], writes=[hk])
                    tr.op("act", lambda e, hb=hb: e.activation(out=junk[:], in_=hb[:], func=AF.Square, accum_out=ssq[:]),
                          reads=[hk], writes=["junk3", "ssq3"])
                    tr.op("act", lambda e: e.activation(out=ssq[:], in_=ssq[:], func=AF.Sqrt, scale=1.0 / D, bias=EPS),
                          reads=["ssq3"], writes=["ssq3"])
                    tr.op("dve", lambda e: e.reciprocal(out=ssq[:], in_=ssq[:]), reads=["ssq3"], writes=["ssq3"])
                    tr.op("dve", lambda e, hb=hb: e.scalar_tensor_tensor(out=hb[:], in0=hb[:], scalar=ssq[:, 0:1], in1=gB[:],
                                                                       op0=ALU.mult, op1=ALU.mult),
                          reads=[hk, "ssq3", "gB3"], writes=[hk])
                    tr.dma("act", hs, out[tt * 128:(tt + 1) * 128, :], hb[:], reads=[hk], writes=["out"])
                tr.barrier()

        if upto >= 1:
            phase_proj(False)
            phase_proj(True)
        if upto >= 3:
            with ExitStack() as cstack:
                cst = conv_pre(cstack)
                phase_attn(do_vcast=(upto >= 5), hook=lambda: conv_hook(cst, 8))
                phase_conv_mix(cst)
                tr.barrier()
        elif upto >= 2:
            phase_attn(do_vcast=(upto >= 5))
        if upto >= 4:
            phase_wout()
        if upto >= 5:
            phase_route()
        if upto >= 6:
            tr.barrier(join_bg=True)
            phase_peer()

        tr.finish("sp")
    return nc


def _rel_bucket_np(rel):
    rel = np.asarray(rel).astype(np.int32)
    n = np.abs(rel)
    nf = np.maximum(n, 1).astype(np.float32)
    large = 8 + (np.log(nf / np.float32(8)) / np.float32(math.log(16)) * np.float32(8)).astype(np.int32)
    large = np.minimum(large, 15)
    return (rel > 0).astype(np.int32) * 16 + np.where(n < 8, n, large)


def make_in_maps(inputs, names=None):
    f = lambda a: np.ascontiguousarray(np.asarray(a, dtype=np.float32))
    x = f(inputs["x"])
    def chunked(w, nk):
        w = np.asarray(w, dtype=np.float32)
        nch = w.shape[1] // 256
        return np.ascontiguousarray(w.reshape(nk, 128, nch, 256).transpose(2, 1, 0, 3))

    def pk(v):
        v = np.asarray(v, dtype=np.float32)
        return np.ascontiguousarray(v.reshape(-1, 128).T)
    shared = {
        "w_in": chunked(inputs["w_in"][0], 16), "norm_mix_g": f(inputs["norm_mix_g"][0]), "b_gate": pk(inputs["b_gate"][0]),
        "lam4": f(np.stack([np.asarray(inputs[k][0]) for k in ("lam_q1", "lam_k1", "lam_q2", "lam_k2")])),
        "subln_g": f(inputs["subln_g"][0]), "w_att_out": chunked(inputs["w_att_out"][0], 8),
        "conv_w": np.ascontiguousarray(np.asarray(inputs["conv_w"][0], dtype=np.float32).reshape(31, 8, 128).transpose(2, 1, 0)),
        "cvec_in": np.ascontiguousarray(np.stack([pk(inputs[k][0]) for k in ("conv_b", "conv_ln_g", "conv_ln_b")], axis=1)),
        "w_conv_out": chunked(inputs["w_conv_out"][0], 8), "w_out": f(inputs["w_out"][0]),
        "rel_bias": f(inputs["rel_bias"]), "norm_ffn_g": f(inputs["norm_ffn_g"][0]),
        "peer_w_q": f(inputs["peer_w_q"][0]),
        "peer_sub_keys": f(np.asarray(inputs["peer_sub_keys"][0]).reshape(16, 128, 128)),
        "peer_u": f(inputs["peer_u"][0]), "peer_v": f(inputs["peer_v"][0]),
        "final_norm_g": f(inputs["final_norm_g"]),
    }
    rp = np.arange(512)
    bk = _rel_bucket_np(256 - rp)
    oh = np.zeros((32, 512), np.float32)
    oh[bk, rp] = 1.0
    shared["oh_r"] = oh
    shared["iota128"] = np.tile(np.arange(128, dtype=np.float32)[None, :], (128, 1))
    maps = []
    for core in range(8):
        b, s = core // 2, core % 2
        m = dict(shared)
        m["x_own"] = np.ascontiguousarray(x[b, s * T:(s + 1) * T])
        m["x_pre"] = np.ascontiguousarray(x[b, 0:T])
        fl = np.zeros((128, 2), np.float32)
        fl[:, 0] = 1.0 if s == 1 else 0.0
        fl[:, 1] = 0.0 if s == 1 else NEG
        m["flags"] = fl
        if names is not None:
            m = {k: v for k, v in m.items() if k in names}
        maps.append(m)
    return maps


def kernel(**inputs):
    nc = build()
    in_maps = make_in_maps(inputs)
    used = set(nc._used_inputs.keys())
    in_maps = [{k: v for k, v in m.items() if k in used} for m in in_maps]
    res = run_bass_kernel_spmd(nc, in_maps, core_ids=list(range(8)))
    outp = np.zeros((4, 4096, D), np.float32)
    for core in range(8):
        b, s = core // 2, core % 2
        outp[b, s * T:(s + 1) * T] = res.results[core]["out"]
    return outp
```
